# Optimizing a Trainium2 kernel written in Bass

```python
import math
import jax, jax.numpy as jnp
from jax import lax
import numpy as np

D_MODEL = 2048
BATCH = 2
SEQ = 4096
DEPTH = 1

GRID_W = 64
CTX_LEN = 256
RET_HEADS = 4
RET_HEAD_DIM = 256
RET_WIDTH = RET_HEADS * RET_HEAD_DIM
NA_HEADS = 8
NA_HEAD_DIM = 128
NA_WIDTH = NA_HEADS * NA_HEAD_DIM
MIX_WIDTH = RET_WIDTH + NA_WIDTH
IN_PROJ_WIDTH = 4 * RET_WIDTH + 3 * NA_WIDTH
RET_CHUNK = 128
NA_ROWS = 8
NA_COLS = 16
ROPE_BASE = 10000.0
N_GROUPS = 4
EXPERTS_PER_GROUP = 8
N_EXPERTS = N_GROUPS * EXPERTS_PER_GROUP
TOP_K_EXPERTS = 2
EXPERT_FF = 512
N_MOD = 6
NORM_EPS = 1e-6

kernel_name = "hymba_style_retention_natten_hmoe_dit"


def rmsnorm(x, w):
    xf = x.astype(jnp.float32)
    y = xf * lax.rsqrt(jnp.mean(xf * xf, axis=-1, keepdims=True) + NORM_EPS)
    return (y * w.astype(jnp.float32)).astype(x.dtype)


def modulate(h, shift, scale):
    return h * (1.0 + scale) + shift


def heads(t, n_heads):
    return t.reshape(t.shape[:-1] + (n_heads, t.shape[-1] // n_heads))


def bhnd(t):
    return jnp.swapaxes(t, 1, 2).astype(jnp.float32)


def split_proj(p):
    cuts = [RET_WIDTH, 2 * RET_WIDTH, 3 * RET_WIDTH, 4 * RET_WIDTH,
            4 * RET_WIDTH + NA_WIDTH, 4 * RET_WIDTH + 2 * NA_WIDTH]
    return jnp.split(p, cuts, axis=-1)


def rope_1d(x, pos):
    d = x.shape[-1]
    inv = ROPE_BASE ** (-jnp.arange(0, d, 2, dtype=jnp.float32) / d)
    ang = pos.astype(jnp.float32)[:, None] * inv[None, :]
    cos = jnp.cos(ang)[None, :, None, :]
    sin = jnp.sin(ang)[None, :, None, :]
    x1, x2 = x[..., : d // 2], x[..., d // 2:]
    return jnp.concatenate([x1 * cos - x2 * sin, x1 * sin + x2 * cos], axis=-1).astype(x.dtype)


def rope_2d(x, rows, cols):
    half = x.shape[-1] // 2
    return jnp.concatenate([rope_1d(x[..., :half], rows), rope_1d(x[..., half:], cols)], axis=-1)


def retention_chunked(q, k, v, log_gamma, state0):
    b, h, n, _ = q.shape
    dv = v.shape[-1]
    nc = n // RET_CHUNK
    pos = jnp.arange(RET_CHUNK, dtype=jnp.float32)
    diff = pos[:, None] - pos[None, :]
    lg = log_gamma[:, None, None]
    intra = jnp.where(diff >= 0, jnp.exp(lg * jnp.maximum(diff, 0.0)), 0.0)
    q_dec = jnp.exp(log_gamma[:, None] * (pos + 1.0))[..., None]
    k_dec = jnp.exp(log_gamma[:, None] * (RET_CHUNK - 1.0 - pos))[..., None]
    c_dec = jnp.exp(log_gamma * RET_CHUNK)[:, None, None]

    def chunks(t):
        return jnp.moveaxis(t.reshape(b, h, nc, RET_CHUNK, t.shape[-1]), 2, 0)

    def step(state, qkv):
        qi, ki, vi = qkv
        scores = jnp.einsum('bhid,bhjd->bhij', qi, ki) * intra
        out = (jnp.einsum('bhij,bhjv->bhiv', scores, vi)
               + jnp.einsum('bhid,bhdv->bhiv', qi * q_dec, state))
        state = state * c_dec + jnp.einsum('bhjd,bhjv->bhdv', ki * k_dec, vi)
        return state, out

    state, out = lax.scan(step, state0, (chunks(q), chunks(k), chunks(v)))
    return jnp.moveaxis(out, 0, 2).reshape(b, h, n, dv), state


def retention_bidir(q, k, v, lg_f, lg_b, s0_f, s0_b):
    o_f, s_f = retention_chunked(q, k, v, lg_f, s0_f)
    o_b, s_b = retention_chunked(jnp.flip(q, 2), jnp.flip(k, 2), jnp.flip(v, 2), lg_b, s0_b)
    return o_f + jnp.flip(o_b, 2), s_f, s_b


def retention_final_state(k, v, log_gamma, reverse):
    n = k.shape[2]
    pos = jnp.arange(n, dtype=jnp.float32)
    expo = pos if reverse else (n - 1.0 - pos)
    w = jnp.exp(log_gamma[:, None] * expo)[..., None]
    return jnp.einsum('bhjd,bhjv->bhdv', k * w, v)


def retention_output(o, gn_w, g):
    b, h, n, dv = o.shape
    mu = jnp.mean(o, axis=-1, keepdims=True)
    var = jnp.mean(jnp.square(o - mu), axis=-1, keepdims=True)
    o = (o - mu) * lax.rsqrt(var + NORM_EPS) * gn_w.astype(jnp.float32).reshape(h, 1, dv)
    o = jnp.swapaxes(o, 1, 2).reshape(b, n, h * dv)
    return (o * jax.nn.silu(g.astype(jnp.float32))).astype(g.dtype)


def neighbourhood_attention(q, k, v, kc, vc, rpb):
    b, n, h, d = q.shape
    rows = n // GRID_W
    win_r = min(NA_ROWS, rows)
    n_keys = win_r * NA_COLS
    scale = d ** -0.5
    qg = (q * scale).reshape(b, rows, GRID_W, h, d)
    kg = k.reshape(b, rows, GRID_W, h, d)
    vg = v.reshape(b, rows, GRID_W, h, d)
    kc_s = kc
    col = jnp.arange(GRID_W)
    col_start = jnp.clip(col - NA_COLS // 2, 0, GRID_W - NA_COLS)
    col_idx = col_start[:, None] + jnp.arange(NA_COLS)[None, :]
    col_off = col_idx - col[:, None] + (NA_COLS - 1)

    def gather_window(t, rs):
        band = lax.dynamic_slice_in_dim(t, rs, win_r, axis=1)
        win = band[:, :, col_idx]
        return jnp.moveaxis(win, 1, 2).reshape(b, GRID_W, n_keys, h, d)

    def row_fn(r):
        rs = jnp.clip(r - NA_ROWS // 2, 0, rows - win_r)
        qr = lax.dynamic_index_in_dim(qg, r, axis=1, keepdims=False)
        kw = gather_window(kg, rs)
        vw = gather_window(vg, rs)
        row_off = rs + jnp.arange(win_r) - r + (NA_ROWS - 1)
        bias = rpb[:, row_off[:, None, None], col_off[None, :, :]]
        bias = jnp.moveaxis(bias, 1, 2).reshape(h, GRID_W, n_keys)
        s_win = jnp.einsum('bqhd,bqkhd->bhqk', qr, kw) + bias[None]
        s_ctx = jnp.einsum('bqhd,bkhd->bhqk', qr, kc_s)
        p = jax.nn.softmax(jnp.concatenate([s_win, s_ctx], axis=-1).astype(jnp.float32), axis=-1).astype(v.dtype)
        return (jnp.einsum('bhqk,bqkhd->bqhd', p[..., :n_keys], vw)
                + jnp.einsum('bhqk,bkhd->bqhd', p[..., n_keys:], vc))

    out = lax.map(row_fn, jnp.arange(rows))
    return jnp.moveaxis(out, 0, 1).reshape(b, n, h * d)


def context_attention(q, k, v):
    b, l, h, d = q.shape
    s = jnp.einsum('bqhd,bkhd->bhqk', q * d ** -0.5, k)
    p = jax.nn.softmax(s.astype(jnp.float32), axis=-1).astype(v.dtype)
    return jnp.einsum('bhqk,bkhd->bqhd', p, v).reshape(b, l, h * d)


def hierarchical_moe(h, w_rg, b_rg, w_re, b_re, w_gate, w_up, w_down):
    g_logits = (h @ w_rg + b_rg).astype(jnp.float32)
    g_prob = jax.nn.softmax(g_logits, axis=-1)
    g_sel = jnp.argmax(g_logits, axis=-1)
    g_w = jnp.take_along_axis(g_prob, g_sel[:, None], axis=-1)
    e_logits = (jnp.einsum('td,gde->tge', h, w_re) + b_re).astype(jnp.float32)
    e_logits = jnp.take_along_axis(e_logits, g_sel[:, None, None], axis=1)[:, 0]
    top_v, top_i = lax.top_k(e_logits, TOP_K_EXPERTS)
    e_w = jax.nn.softmax(top_v, axis=-1) * g_w
    expert_id = g_sel[:, None] * EXPERTS_PER_GROUP + top_i
    combine = jnp.sum(jax.nn.one_hot(expert_id, N_EXPERTS, dtype=jnp.float32) * e_w[..., None], axis=1)
    combine = combine.astype(h.dtype)
    out = jnp.zeros_like(h)
    for e in range(N_EXPERTS):
        a = jax.nn.silu(h @ w_gate[e]) * (h @ w_up[e])
        out = out + combine[:, e:e + 1] * (a @ w_down[e])
    return out


def trunk_layer(x, xc, c, c_ctx, w_mod, b_mod, norm_mix_w, w_in, ret_decay_f, ret_decay_b,
                ret_gn_w, na_rpb, w_out, norm_ffn_w, w_rg, b_rg, w_re, b_re,
                w_gate, w_up, w_down, last):
    b, n, d_model = x.shape
    mod = jax.nn.silu(c) @ w_mod + b_mod
    sh_a, sc_a, g_a, sh_f, sc_f, g_f = [m[:, None, :] for m in jnp.split(mod, N_MOD, axis=-1)]
    mod_c = jax.nn.silu(c_ctx) @ w_mod + b_mod
    csh_a, csc_a, cg_a, csh_f, csc_f, cg_f = jnp.split(mod_c, N_MOD, axis=-1)

    hx = modulate(rmsnorm(x, norm_mix_w), sh_a, sc_a)
    hc = modulate(rmsnorm(xc, norm_mix_w), csh_a, csc_a)
    rq, rk, rv, rg, nq, nk, nv = split_proj(hx @ w_in)
    crq, crk, crv, crg, cnq, cnk, cnv = split_proj(hc @ w_in)

    t = jnp.arange(n)
    rows_pos, cols_pos = t // GRID_W, t % GRID_W
    lg_f = jax.nn.log_sigmoid(ret_decay_f.astype(jnp.float32))
    lg_b = jax.nn.log_sigmoid(ret_decay_b.astype(jnp.float32))
    k_scale = RET_HEAD_DIM ** -0.5

    ck = bhnd(heads(crk, RET_HEADS)) * k_scale
    cv = bhnd(heads(crv, RET_HEADS))
    if last:
        s_f = retention_final_state(ck, cv, lg_f, False)
        s_b = retention_final_state(ck, cv, lg_b, True)
    else:
        zeros = jnp.zeros((b, RET_HEADS, RET_HEAD_DIM, RET_HEAD_DIM), jnp.float32)
        oc, s_f, s_b = retention_bidir(bhnd(heads(crq, RET_HEADS)), ck, cv, lg_f, lg_b, zeros, zeros)
    q = bhnd(rope_2d(heads(rq, RET_HEADS), rows_pos, cols_pos))
    k = bhnd(rope_2d(heads(rk, RET_HEADS), rows_pos, cols_pos)) * k_scale
    v = bhnd(heads(rv, RET_HEADS))
    o, _, _ = retention_bidir(q, k, v, lg_f, lg_b, s_f, s_b)
    ret_x = retention_output(o, ret_gn_w, rg)

    kc_na = heads(cnk, NA_HEADS)
    vc_na = heads(cnv, NA_HEADS)
    na_x = neighbourhood_attention(heads(nq, NA_HEADS), heads(nk, NA_HEADS), heads(nv, NA_HEADS),
                                   kc_na, vc_na, na_rpb)
    x = x + g_a * (jnp.concatenate([ret_x, na_x], axis=-1) @ w_out)

    if not last:
        ret_c = retention_output(oc, ret_gn_w, crg)
        na_c = context_attention(heads(cnq, NA_HEADS), kc_na, vc_na)
        xc = xc + cg_a * (jnp.concatenate([ret_c, na_c], axis=-1) @ w_out)
        hcf = modulate(rmsnorm(xc, norm_ffn_w), csh_f, csc_f)
        xc = xc + cg_f * hierarchical_moe(hcf.reshape(-1, d_model), w_rg, b_rg, w_re, b_re,
                                          w_gate, w_up, w_down).reshape(xc.shape)

    hf = modulate(rmsnorm(x, norm_ffn_w), sh_f, sc_f)
    x = x + g_f * hierarchical_moe(hf.reshape(-1, d_model), w_rg, b_rg, w_re, b_re,
                                   w_gate, w_up, w_down).reshape(x.shape)
    return x, xc


def setup_inputs(seed: int = 0) -> dict:
    key = jax.random.key(seed)
    ks = jax.random.split(key, 24)
    D = D_MODEL
    nrm = jax.random.normal
    ladder = jnp.log(2.0 ** (5.0 + jnp.arange(RET_HEADS, dtype=jnp.float32)) - 1.0)
    return {
        "x": nrm(ks[0], (BATCH, SEQ, D), jnp.float32),
        "c": nrm(ks[1], (BATCH, D), jnp.float32),
        "ctx": nrm(ks[2], (BATCH, CTX_LEN, D), jnp.float32),
        "c_ctx": nrm(ks[3], (D,), jnp.float32),
        "w_mod": nrm(ks[4], (DEPTH, D, N_MOD * D), jnp.float32) * (0.5 * D ** -0.5),
        "b_mod": nrm(ks[5], (DEPTH, N_MOD * D), jnp.float32) * 0.01,
        "norm_mix_w": 1.0 + 0.05 * nrm(ks[6], (DEPTH, D), jnp.float32),
        "w_in": nrm(ks[7], (DEPTH, D, IN_PROJ_WIDTH), jnp.float32) * D ** -0.5,
        "ret_decay_f": ladder + 0.05 * nrm(ks[8], (DEPTH, RET_HEADS), jnp.float32),
        "ret_decay_b": ladder + 0.05 * nrm(ks[9], (DEPTH, RET_HEADS), jnp.float32),
        "ret_gn_w": 1.0 + 0.05 * nrm(ks[10], (DEPTH, RET_WIDTH), jnp.float32),
        "na_rpb": 0.1 * nrm(ks[11], (DEPTH, NA_HEADS, 2 * NA_ROWS - 1, 2 * NA_COLS - 1), jnp.float32),
        "w_out": nrm(ks[12], (DEPTH, MIX_WIDTH, D), jnp.float32) * MIX_WIDTH ** -0.5,
        "norm_ffn_w": 1.0 + 0.05 * nrm(ks[13], (DEPTH, D), jnp.float32),
        "w_router_group": nrm(ks[14], (DEPTH, D, N_GROUPS), jnp.float32) * D ** -0.5,
        "b_router_group": 0.01 * nrm(ks[15], (DEPTH, N_GROUPS), jnp.float32),
        "w_router_expert": nrm(ks[16], (DEPTH, N_GROUPS, D, EXPERTS_PER_GROUP), jnp.float32) * D ** -0.5,
        "b_router_expert": 0.01 * nrm(ks[17], (DEPTH, N_GROUPS, EXPERTS_PER_GROUP), jnp.float32),
        "w_gate": nrm(ks[18], (DEPTH, N_EXPERTS, D, EXPERT_FF), jnp.float32) * D ** -0.5,
        "w_up": nrm(ks[19], (DEPTH, N_EXPERTS, D, EXPERT_FF), jnp.float32) * D ** -0.5,
        "w_down": nrm(ks[20], (DEPTH, N_EXPERTS, EXPERT_FF, D), jnp.float32) * EXPERT_FF ** -0.5,
        "final_norm_w": 1.0 + 0.05 * nrm(ks[21], (D,), jnp.float32),
    }


def reference(x, c, ctx, c_ctx, w_mod, b_mod, norm_mix_w, w_in, ret_decay_f, ret_decay_b,
              ret_gn_w, na_rpb, w_out, norm_ffn_w, w_router_group, b_router_group,
              w_router_expert, b_router_expert, w_gate, w_up, w_down, final_norm_w):
    xc = ctx
    for l in range(DEPTH):
        x, xc = trunk_layer(x, xc, c, c_ctx, w_mod[l], b_mod[l], norm_mix_w[l], w_in[l],
                            ret_decay_f[l], ret_decay_b[l], ret_gn_w[l], na_rpb[l], w_out[l],
                            norm_ffn_w[l], w_router_group[l], b_router_group[l],
                            w_router_expert[l], b_router_expert[l], w_gate[l], w_up[l], w_down[l],
                            last=(l == DEPTH - 1))
    return rmsnorm(x, final_norm_w)
```

```python
import numpy as np
import concourse.bass as bass
import concourse.mybir as mybir
from concourse.bass_utils import run_bass_kernel_spmd

F32 = mybir.dt.float32
BF16 = mybir.dt.bfloat16
AF = mybir.ActivationFunctionType
ALU = mybir.AluOpType
AX = mybir.AxisListType

NEG = -30000.0
EPS = 1e-6
SB_BASE = 16640
SB_END = 16512 + 212863

DEBUG = {}


class Sch:
    def __init__(self, nc, n_dma_sems=40):
        self.nc = nc
        self.E = {'pe': nc.tensor, 'act': nc.scalar, 'dve': nc.vector, 'pool': nc.gpsimd, 'sp': nc.sync}
        self.ops = []
        self.lastw = {}
        self.readers = {}
        self.bar_deps = {}
        self.open_async = set()
        self.last_on = {}
        self.n_dma_sems = n_dma_sems

    def op(self, eng, fn, r=(), w=(), kind='c'):
        i = len(self.ops)
        deps = {}
        psr = [k_ for k_ in r if isinstance(k_, tuple) and len(k_) == 2 and k_[0] == 'ps']
        if psr:
            r = [k_ for k_ in r if k_ not in psr]
            w = list(w) + [k_ for k_ in psr if k_ not in w]

        def add(d, k):
            if d is None or d == i:
                return
            if d in deps and (deps[d] != 'war' or k == 'war'):
                return
            deps[d] = k

        for b in r:
            add(self.lastw.get(b), 'raw')
        for b in w:
            add(self.lastw.get(b), 'waw')
            rd = self.readers.get(b)
            if rd:
                for d in rd[0].values():
                    add(d, 'war')
                for d in rd[1]:
                    add(d, 'war')
        bd = self.bar_deps.pop(eng, None)
        if bd:
            for d in bd:
                add(d, 'raw')
        for b in w:
            self.lastw[b] = i
            self.readers[b] = ({}, [])
        for b in r:
            rd = self.readers.setdefault(b, ({}, []))
            if kind == 'c':
                rd[0][eng] = i
            else:
                rd[1].append(i)
        for d in deps:
            self.open_async.discard(d)
        if kind != 'c':
            self.open_async.add(i)
        self.ops.append((eng, fn, deps, kind))
        self.last_on[eng] = i
        return i

    def bar(self):
        deps = set(self.last_on.values()) | set(self.open_async)
        self.open_async.clear()
        for e in self.E:
            self.bar_deps[e] = set(deps) | self.bar_deps.get(e, set())

    def emit(self, stack):
        nc = self.nc
        ops = self.ops
        n = len(ops)
        need = [False] * n
        for i, (eng, fn, deps, kind) in enumerate(ops):
            for d, k in deps.items():
                pe_, _, _, pk = ops[d]
                if pk != 'c':
                    continue
                if pe_ == eng and eng == 'pe':
                    continue
                need[d] = True
        psem = {e: stack.enter_context(nc.semaphore("pg_" + e)) for e in self.E}
        half_n = self.n_dma_sems // 2
        dsem = [stack.enter_context(nc.semaphore("dm_%d" % j)) for j in range(self.n_dma_sems)]
        duse = [0] * self.n_dma_sems
        ndma_q = {'pool': 0, 'other': 0}
        pcnt = {e: 0 for e in self.E}
        sig = [None] * n
        waited = {e: {} for e in self.E}
        ndma = 0
        for i, (eng, fn, deps, kind) in enumerate(ops):
            E = self.E[eng]
            wl = {}
            for d, k in deps.items():
                pe_, _, _, pk = ops[d]
                if pk == 'c' and pe_ == eng and eng == 'pe':
                    continue
                s, v = sig[d]
                key = id(s)
                if waited[eng].get(key, 0) >= v:
                    continue
                if key not in wl or wl[key][1] < v:
                    wl[key] = (s, v)
            my = None
            if kind == 'd':
                qk = 'pool' if eng == 'pool' else 'other'
                j = (ndma_q[qk] % half_n) + (0 if qk == 'pool' else half_n)
                ndma_q[qk] += 1
                ndma += 1
                s = dsem[j]
                if duse[j] > 0:
                    key = id(s)
                    pv = 16 * duse[j]
                    if waited[eng].get(key, 0) < pv and (key not in wl or wl[key][1] < pv):
                        wl[key] = (s, pv)
                duse[j] += 1
                my = (s, 16 * duse[j], 16)
            elif kind == 'cc':
                s = stack.enter_context(nc.semaphore("cc_%d" % i))
                my = (s, 1, 1)
            elif need[i]:
                pcnt[eng] += 1
                my = (psem[eng], pcnt[eng], 1)
            for key, (s, v) in wl.items():
                E.wait_ge(s, v)
                waited[eng][key] = v
            ins = fn(E)
            if my is not None:
                ins.then_inc(my[0], my[2])
                sig[i] = (my[0], my[1])
        return


class Arena:
    def __init__(self, nc):
        self.nc = nc
        self.off = SB_BASE
        self.n = 0

    def alloc(self, shape, dtype, name=None):
        sz = 1
        for s in shape[1:]:
            sz *= s
        sz *= 2 if dtype == BF16 else 4
        off = (self.off + 63) // 64 * 64
        assert off + sz <= min(SB_END, getattr(self, 'limit', SB_END)), ("SBUF overflow", name, off, sz, self.limit)
        self.n += 1
        t = self.nc.alloc_sbuf_tensor_at("%s_%d" % (name or "t", self.n), list(shape), dtype, offset=off)
        self.off = off + sz
        return t

    def seek(self, kb, limit_kb):
        self.off = SB_BASE + int(kb * 1024)
        self.limit = SB_BASE + int(limit_kb * 1024)


def nl_list(m):
    return list(range(m, m + 6)) if m <= 6 else list(range(6, 12))


def build_program(stop=None, dbg=()):
    nc = bass.Bass("TRN2", target_bir_lowering=False)
    S = Sch(nc)
    A = Arena(nc)

    _shapes = {"xs": [1536, 2048], "ctxb": [256, 2048], "colp": [128, 160], "wmod": [2048, 12288],
               "win": [2048, 7168], "wout": [2048, 2048], "wrt": [2048, 36], "brt": [36], "wg": [32, 2048, 512],
               "wu": [32, 2048, 512], "wd": [32, 512, 2048], "fnw": [2048], "gnw": [1024], "dec": [8],
               "ropet": [128, 2, 1024], "tcd": [8, 8, 128, 6, 128], "rtab": [128, 30], "dmk": [128, 4, 128]}
    _decl = {}

    class _Lazy:
        def __init__(self, name):
            self.name = name

        def ap(self):
            if self.name not in _decl:
                _decl[self.name] = nc.dram_tensor(self.name, list(_shapes[self.name]), F32, kind="ExternalInput").ap()
            return _decl[self.name]

        def __getitem__(self, key):
            return self.ap()[key]

        def rearrange(self, *a, **k):
            return self.ap().rearrange(*a, **k)

        def partition_broadcast(self, n):
            return self.ap().partition_broadcast(n)

    xs, ctxb, colp_d, wmod, win, wout, wrt, brt, wg, wu, wd, fnw, gnw, dec, ropet_d, tcd, rtab_d, dmk_d = [
        _Lazy(n) for n in ["xs", "ctxb", "colp", "wmod", "win", "wout", "wrt", "brt", "wg", "wu", "wd", "fnw", "gnw",
                           "dec", "ropet", "tcd", "rtab", "dmk"]]
    nc._declared_inputs = _decl
    y = nc.dram_tensor("y", [1024, 2048], F32, kind="ExternalOutput").ap()
    x1s = nc.dram_tensor("x1s", [1024, 2048], F32).ap()
    stin = [nc.dram_tensor("stin%d" % h, [512, 256], F32) for h in range(4)]
    stall = [nc.dram_tensor("stall%d" % h, [2048, 256], F32) for h in range(4)]
    dbg_n = [0]

    def dump(name, ap, shape, keys, dtype=F32):
        if name not in dbg:
            return
        d = nc.dram_tensor("dbg_" + name, list(shape), dtype, kind="ExternalOutput").ap()
        S.op('sp', lambda e: e.dma_start(out=d, in_=ap), r=keys, w=[('dbg', name)], kind='d')

    def finish():
        S.bar()
        S.op('sp', lambda e: e.nop(), r=[], w=[])
        from contextlib import ExitStack
        with ExitStack() as es:
            S.emit(es)
        return nc

    PS = nc.alloc_psum_tensor("PS", [128, 8, 512], F32)

    def psb(b):
        return PS[:, b, :]

    def pskey(b):
        return ('ps', b)

    A.seek(0, 6)
    ident = A.alloc([128, 128], BF16, "ident")
    identf = A.alloc([128, 128], F32, "identf")
    colp = A.alloc([128, 160], F32, "colp")
    modc = A.alloc([128, 96, 2], F32, "modc")
    ABc = A.alloc([128, 3, 16], F32, "ABc")
    sc = A.alloc([128, 16, 2], BF16, "sc")
    small = A.alloc([128, 64], F32, "small")
    rsm = A.alloc([128, 64], F32, "rsm")
    comb = A.alloc([128, 8, 32], F32, "comb")
    wrb = A.alloc([128, 16, 36], BF16, "wrb")
    brb = A.alloc([128, 36], F32, "brb")
    lgt = A.alloc([128, 36], F32, "lgt")
    A.seek(6, 38)
    cat = A.alloc([128, 8, 2048], BF16, "cat")

    S.op('pool', lambda e: e.memset(ident[:], 1.0), w=['ident'])
    S.op('pool', lambda e: e.affine_select(out=ident[:], in_=ident[:], pattern=[[-1, 128]], compare_op=ALU.is_equal,
                                           fill=0.0, base=0, channel_multiplier=1), r=['ident'], w=['ident'])
    S.op('pool', lambda e: e.memset(identf[:], 1.0), w=['identf'])
    S.op('pool', lambda e: e.affine_select(out=identf[:], in_=identf[:], pattern=[[-1, 128]], compare_op=ALU.is_equal,
                                           fill=0.0, base=0, channel_multiplier=1), r=['identf'], w=['identf'])
    S.op('sp', lambda e: e.dma_start(out=colp[:], in_=colp_d.ap()), w=['colp'], kind='d')
    ccol = colp[:, 0:32]
    bmodc = colp[:, 32:128]
    nw1c = colp[:, 128:144]
    nw2c = colp[:, 144:160]
    S.op('act', lambda e: e.activation(out=sc[:].rearrange("p k c -> p (k c)"), in_=ccol, func=AF.Silu),
         r=['colp'], w=['sc'])

    A.seek(38, 126)
    hTa = A.alloc([128, 16, 1280], BF16, "hTa")
    ring = [A.alloc([128, 16, 256], BF16, "ring%d" % i) for i in range(4)]
    hTb = A.alloc([128, 16, 512], BF16, "hTb")
    A.seek(126, 175.5)

    def hloc(t):
        if 2 <= t <= 9:
            return hTa, (t - 2) * 128
        if t >= 12:
            return hTa, 1024 + (t - 12) * 128
        if t < 2:
            return hTb, t * 128
        return hTb, (t - 8) * 128

    def hTk(t, k):
        H, c0 = hloc(t)
        return H[:, k, c0:c0 + 128]

    if stop == '0':
        dump('sc', sc[:], [128, 16, 2], ['sc'], BF16)
        return finish()
    A.seek(175.5, 207.5)
    mring = [A.alloc([128, 16, 256], BF16, "mring%d" % i) for i in range(4)]
    A.seek(126, 175.5)

    def modA_dma(t):
        sl, key = (ring[t % 4], 'ring') if t < 16 else (mring[t % 4], 'mring')
        S.op('pool', lambda e, sl=sl, t=t: e.dma_start(
            out=sl[:], in_=wmod[:, t * 256:(t + 1) * 256].rearrange("(k p) n -> p k n", p=128)),
            w=[(key, t % 4, 0), (key, t % 4, 1)], kind='d')

    def modA_mm(t):
        sl, key = (ring[t % 4], 'ring') if t < 16 else (mring[t % 4], 'mring')
        for jj in range(2):
            j = 2 * t + jj
            bank = 0 if j < 32 else 6
            jl = j if j < 32 else j - 32 + 128
            for k in range(16):
                S.op('pe', lambda e, sl=sl, jj=jj, k=k, bank=bank, jl=jl: e.matmul(
                    psb(bank)[:, jl * 2:jl * 2 + 2], lhsT=sl[:, k, jj * 128:(jj + 1) * 128], rhs=sc[:, k, :],
                    start=(k == 0), stop=(k == 15)),
                    r=[(key, t % 4, jj), 'sc'], w=[pskey(bank)])

    def modA_tail_finish():
        S.op('dve', lambda e: e.tensor_tensor(
            out=modc[:, 32:96, 0], in0=psb(6)[:, 256:384].rearrange("p (j c) -> p j c", c=2)[:, :, 0],
            in1=bmodc[:, 32:96], op=ALU.add), r=[pskey(6), 'colp'], w=[('modc', 1, 0)])
        S.op('dve', lambda e: e.scalar_tensor_tensor(out=ABc[:, 2, :], in0=modc[:, 64:80, 0], scalar=1.0, in1=nw2c,
                                                     op0=ALU.add, op1=ALU.mult),
             r=[('modc', 1, 0), 'colp'], w=['A2'])

    nA = 16
    for t in range(nA):
        modA_dma(t)
        modA_mm(t)
        if t == 15:
            for c in range(2):
                S.op('dve', lambda e, c=c: e.tensor_tensor(
                    out=modc[:, 0:32, c], in0=psb(0)[:, 0:64].rearrange("p (j c) -> p j c", c=2)[:, :, c],
                    in1=bmodc[:, 0:32], op=ALU.add), r=[pskey(0), 'colp'], w=[('modc', 0, c)])
            S.op('dve', lambda e: e.scalar_tensor_tensor(out=ABc[:, 0, :], in0=modc[:, 16:32, 0], scalar=1.0, in1=nw1c,
                                                         op0=ALU.add, op1=ALU.mult),
                 r=[('modc', 0, 0), 'colp'], w=['A1'])
            S.op('dve', lambda e: e.scalar_tensor_tensor(out=ABc[:, 1, :], in0=modc[:, 16:32, 1], scalar=1.0, in1=nw1c,
                                                         op0=ALU.add, op1=ALU.mult),
                 r=[('modc', 0, 1), 'colp'], w=['Ac'])
    dump('modc', modc[:], [128, 96, 2], [('modc', 0, 0), ('modc', 0, 1), ('modc', 1, 0)])
    if stop == 'A':
        for t in range(16, 48):
            modA_dma(t)
            modA_mm(t)
        modA_tail_finish()
        return finish()
    xin = [A.alloc([128, 2048], F32, "xin%d" % i) for i in range(2)]
    xn = [A.alloc([128, 2048], BF16, "xn%d" % i) for i in range(2)]
    ss = A.alloc([128, 16], F32, "ss")
    rs = A.alloc([128, 16], F32, "rs")
    S.op('pool', lambda e: e.memset(ss[:], 0.0), w=['ss'])

    def rms_tile(junk_t, junk_key, xin_t, xin_key, ss_col, rs_col, tag):
        S.op('act', lambda e: e.activation(out=junk_t[:], in_=xin_t[:], func=AF.Square, accum_out=ss_col),
             r=[xin_key, 'ss'], w=[junk_key, (tag, 'ss')])
        S.op('dve', lambda e: e.tensor_scalar(out=rs_col, in0=ss_col, scalar1=1.0 / 2048, scalar2=EPS, op0=ALU.mult,
                                              op1=ALU.add), r=[(tag, 'ss')], w=[(tag, 'rs')])
        S.op('act', lambda e: e.activation(out=rs_col, in_=rs_col, func=AF.Sqrt), r=[(tag, 'rs')], w=[(tag, 'rs')])
        S.op('dve', lambda e: e.reciprocal(out=rs_col, in_=rs_col), r=[(tag, 'rs')], w=[(tag, 'rs')])

    def transpose_mod(src_bf, src_key, dst_fn, dst_keys, Acol, Bcol, Akey, Bkey, pbank):
        pst = PS[:, pbank:pbank + 2, :].rearrange("p b f -> p (b f)").bitcast(BF16)
        pst = pst.rearrange("p (k t) -> p k t", t=128)
        for k in range(16):
            S.op('pe', lambda e, k=k: e.transpose(pst[:, k, :], src_bf[:, k * 128:(k + 1) * 128], ident[:]),
                 r=[src_key, 'ident'], w=[pskey(pbank + k // 8)])
        for k in range(16):
            if k < 8:
                S.op('act', lambda e, k=k: e.activation(out=dst_fn(k), in_=pst[:, k, :], func=AF.Identity,
                                                        scale=Acol(k), bias=Bcol(k)),
                     r=[pskey(pbank + k // 8), Akey, Bkey], w=[dst_keys(k)])
            else:
                S.op('dve', lambda e, k=k: e.tensor_scalar(out=dst_fn(k), in0=pst[:, k, :], scalar1=Acol(k),
                                                           scalar2=Bcol(k), op0=ALU.mult, op1=ALU.add),
                     r=[pskey(pbank + k // 8), Akey, Bkey], w=[dst_keys(k)])

    for t in range(14):
        xi = xin[t % 2]
        xk = ('xin', t % 2)
        src = xs[t * 128:(t + 1) * 128, :] if t < 12 else ctxb[(t - 12) * 128:(t - 11) * 128, :]
        S.op('sp', lambda e, xi=xi, src=src: e.dma_start(out=xi[:], in_=src), w=[xk], kind='d')
        xnt = xn[t % 2]
        rms_tile(xnt, ('xn', t % 2), xi, xk, ss[:, t:t + 1], rs[:, t:t + 1], ('B', t))
        S.op('dve', lambda e, xi=xi, xnt=xnt, t=t: e.tensor_scalar(out=xnt[:], in0=xi[:], scalar1=rs[:, t:t + 1],
                                                                   scalar2=None, op0=ALU.mult),
             r=[xk, (('B', t), 'rs')], w=[('xn', t % 2)])
        ai = 0 if t < 12 else 1
        ci = 0 if t < 12 else 1
        transpose_mod(xnt, ('xn', t % 2),
                      lambda k, t=t: hTk(t, k),
                      lambda k, t=t: ('hT', t),
                      lambda k, ai=ai: ABc[:, ai, k:k + 1],
                      lambda k, ci=ci: modc[:, k, ci:ci + 1],
                      'A1' if t < 12 else 'Ac', ('modc', 0, ci), 2 + 2 * (t % 2))

    hT_all = [('hT', t) for t in range(14)]
    dump('hTa', hTa[:], [128, 16, 1280], hT_all, BF16)
    dump('hTb', hTb[:], [128, 16, 512], hT_all, BF16)
    if stop == 'B':
        return finish()
    NA_REGION_END = 175.5
    hT_own = [('hT', t) for t in range(2, 10)]
    A.seek(126, 175.5)
    S.bar()

    qTn = [A.alloc([128, 1024], BF16, "qTn%d" % i) for i in range(2)]
    kTn = [A.alloc([128, 1792], BF16, "kTn%d" % i) for i in range(2)]
    vA = [A.alloc([128, 14, 130], BF16, "vA%d" % i) for i in range(2)]
    tcb = [A.alloc([128, 6, 128], F32, "tcb%d" % i) for i in range(2)]
    tmpS = [A.alloc([128, 6, 128], F32, "tmpS%d" % i) for i in range(2)]
    PT = [A.alloc([128, 8, 128], BF16, "PT%d" % i) for i in range(2)]
    for i in range(2):
        S.op('pool', lambda e, i=i: e.memset(vA[i][:, :, 128:130], 1.0), w=[('vA1', i)])

    rc = [0]
    SC_NA = float(128 ** -0.5)
    def wload(slot, half, col):
        S.op('pool', lambda e: e.dma_start(out=ring[slot][:, :, half * 128:(half + 1) * 128],
                                           in_=win[:, col:col + 128].rearrange("(k p) n -> p k n", p=128)),
             w=[('ring', slot, half)], kind='d')

    def wload_head(hh):
        t0, t1 = (2 * hh) % 4, (2 * hh + 1) % 4
        wload(t0, 0, 4096 + hh * 128)
        wload(t0, 1, 5120 + hh * 128)
        wload(t1, 0, 6144 + hh * 128)

    kgroups = [(hTb, 0, 512, [(0, 0, 256), (1280, 256, 256)]), (hTa, 0, 512, [(256, 0, 512)]),
               (hTa, 512, 512, [(768, 0, 512)]), (hTa, 1024, 256, [(1536, 0, 256)])]

    def inproj_groups(h):
        b = h % 2
        s0 = (2 * h) % 4
        s1 = (2 * h + 1) % 4
        r0k = ('ring', s0, 0)
        r1k = ('ring', s0, 1)
        r2k = ('ring', s1, 0)
        wq = ring[s0][:, :, 0:128]
        wk = ring[s0][:, :, 128:256]
        wv = ring[s1][:, :, 0:128]
        gl = []

        def g_q(half):
            bank = rc[0] % 2
            rc[0] += 1
            for k in range(16):
                S.op('pe', lambda e, k=k, half=half, bank=bank: e.matmul(
                    psb(bank), lhsT=wq[:, k, :], rhs=hTa[:, k, half * 512:(half + 1) * 512],
                    start=(k == 0), stop=(k == 15)), r=[r0k] + hT_own, w=[pskey(bank)])
            S.op('act', lambda e, half=half, bank=bank: e.activation(
                out=qTn[b][:, half * 512:(half + 1) * 512], in_=psb(bank), func=AF.Copy, scale=SC_NA),
                r=[pskey(bank)], w=[('qTn', b)])

        def g_k(H_, c0, n, dsts):
            bank = rc[0] % 2
            rc[0] += 1
            for k in range(16):
                S.op('pe', lambda e, k=k, bank=bank: e.matmul(
                    psb(bank)[:, 0:n], lhsT=wk[:, k, :], rhs=H_[:, k, c0:c0 + n],
                    start=(k == 0), stop=(k == 15)), r=[r1k] + hT_all, w=[pskey(bank)])
            for (d0, s0_, nn) in dsts:
                S.op('dve', lambda e, bank=bank, d0=d0, s0_=s0_, nn=nn: e.tensor_copy(
                    out=kTn[b][:, d0:d0 + nn], in_=psb(bank)[:, s0_:s0_ + nn]), r=[pskey(bank)], w=[('kTn', b)])

        def g_v(g):
            bank = rc[0] % 2
            rc[0] += 1
            nt = 4 if g < 3 else 2
            for tt in range(nt):
                t = g * 4 + tt
                for k in range(16):
                    S.op('pe', lambda e, k=k, t=t, tt=tt, bank=bank: e.matmul(
                        psb(bank)[:, tt * 128:(tt + 1) * 128], lhsT=hTk(t, k), rhs=wv[:, k, :],
                        start=(k == 0), stop=(k == 15)), r=[r2k, ('hT', t)], w=[pskey(bank)])
            S.op('act', lambda e, g=g, bank=bank, nt=nt: e.activation(
                out=vA[b][:, g * 4:g * 4 + nt, 0:128],
                in_=psb(bank)[:, 0:nt * 128].rearrange("p (t c) -> p t c", c=128), func=AF.Copy),
                r=[pskey(bank)], w=[('vA', b)])

        for half in range(2):
            gl.append(lambda half=half: g_q(half))
        for kg in kgroups:
            gl.append(lambda kg=kg: g_k(*kg))
        for g in range(4):
            gl.append(lambda g=g: g_v(g))
        return gl

    def attn_S(h, m):
        b = h % 2
        p2 = (h * 8 + m) % 2
        sb0 = 2 + 2 * p2
        psS = PS[:, sb0:sb0 + 2, :].rearrange("p b (t c) -> p (b t) c", c=128)
        tiles = nl_list(m) + [12, 13]
        S.op('sp', lambda e: e.dma_start(out=tcb[p2][:], in_=tcd[h, m]), w=[('tcb', p2)], kind='d')
        for w_, n in enumerate(tiles):
            S.op('pe', lambda e, w_=w_, n=n: e.matmul(
                psS[:, w_, :], lhsT=kTn[b][:, n * 128:(n + 1) * 128], rhs=qTn[b][:, m * 128:(m + 1) * 128],
                start=True, stop=True), r=[('kTn', b), ('qTn', b)], w=[pskey(sb0 + w_ // 4)])
        S.op('dve', lambda e: e.tensor_tensor(out=tmpS[p2][:, 0:4, :], in0=psS[:, 0:4, :],
                                              in1=tcb[p2][:, 0:4, :], op=ALU.add),
             r=[pskey(sb0), ('tcb', p2)], w=[('tmpS', p2, 0)])
        S.op('dve', lambda e: e.tensor_tensor(out=tmpS[p2][:, 4:6, :], in0=psS[:, 4:6, :],
                                              in1=tcb[p2][:, 4:6, :], op=ALU.add),
             r=[pskey(sb0 + 1), ('tcb', p2)], w=[('tmpS', p2, 1)])
        S.op('act', lambda e: e.activation(out=PT[p2][:, 0:6, :], in_=tmpS[p2][:], func=AF.Exp),
             r=[('tmpS', p2, 0), ('tmpS', p2, 1)], w=[('PT', p2, 0)])
        S.op('act', lambda e: e.activation(out=PT[p2][:, 6:8, :], in_=psS[:, 6:8, :], func=AF.Exp),
             r=[pskey(sb0 + 1)], w=[('PT', p2, 1)])

    def attn_PV(h, m):
        b = h % 2
        p2 = (h * 8 + m) % 2
        pO = psb(6 + p2)
        tiles = nl_list(m) + [12, 13]
        for w_, n in enumerate(tiles):
            S.op('pe', lambda e, w_=w_, n=n: e.matmul(
                pO[:, 0:130], lhsT=PT[p2][:, w_, :], rhs=vA[b][:, n, :], start=(w_ == 0), stop=(w_ == 7)),
                r=[('PT', p2, 0), ('PT', p2, 1), ('vA', b), ('vA1', b)], w=[pskey(6 + p2)])
        rcol = small[:, p2:p2 + 1]
        S.op('dve', lambda e: e.reciprocal(out=rcol, in_=pO[:, 128:129]), r=[pskey(6 + p2)], w=[('rcol', p2)])
        S.op('act', lambda e: e.activation(
            out=cat[:, m, 1024 + h * 128:1024 + (h + 1) * 128], in_=pO[:, 0:128], func=AF.Copy, scale=rcol),
            r=[pskey(6 + p2), ('rcol', p2)], w=[('cat', m)])

    wload_head(0)
    wload_head(1)
    for g_ in inproj_groups(0):
        g_()
    for h in range(8):
        if h + 2 < 8:
            wload_head(h + 2)
        for tt_ in range(4):
            modA_dma(16 + 4 * h + tt_)
        nxt = inproj_groups(h + 1) if h + 1 < 8 else []
        for m in range(8):
            attn_S(h, m)
            for _ in range(2 if m < 2 else 1):
                if nxt:
                    nxt.pop(0)()
            attn_PV(h, m)
        while nxt:
            nxt.pop(0)()
        for tt_ in range(4):
            modA_mm(16 + 4 * h + tt_)

    modA_tail_finish()

    dump('cat', cat[:], [128, 8, 2048], [('cat', m) for m in range(8)], BF16)
    dump('hTb2', hTb[:], [128, 16, 512], hT_all, BF16)
    dump('kTn1', kTn[1][:], [128, 1792], [('kTn', 1)], BF16)
    dump('vA1', vA[1][:], [128, 14, 130], [('vA', 1), ('vA1', 1)], BF16)
    dump('qTn1', qTn[1][:], [128, 1024], [('qTn', 1)], BF16)
    if stop == 'D':
        return finish()
    A.seek(110, 207.5)
    S.bar()

    ropet = A.alloc([128, 2, 1024], F32, "ropet")
    rtab = A.alloc([128, 30], F32, "rtab")
    dmk = A.alloc([128, 4, 128], F32, "dmk")
    decb = A.alloc([128, 8], F32, "decb")
    lg = A.alloc([128, 8], F32, "lg")
    dc = A.alloc([128, 4, 20], F32, "dc")
    gnb = A.alloc([128, 1024], F32, "gnb")
    Dfb = A.alloc([128, 128], F32, "Dfb")
    e12 = A.alloc([128, 2, 128], F32, "e12")
    qT = A.alloc([128, 2, 1024], BF16, "qT")
    kT = A.alloc([128, 2, 1024], BF16, "kT")
    ktm = [A.alloc([128, 8, 256], BF16, "ktm%d" % d) for d in range(2)]
    vtm = A.alloc([128, 8, 256], BF16, "vtm")
    gs = A.alloc([128, 8, 256], BF16, "gs")
    cktm = [A.alloc([128, 2, 256], BF16, "cktm%d" % d) for d in range(2)]
    cvtm = A.alloc([128, 2, 256], BF16, "cvtm")
    St = [A.alloc([128, 2, 256], F32, "St%d" % d) for d in range(2)]
    Sbf = [A.alloc([128, 8, 2, 256], BF16, "Sbf%d" % d) for d in range(2)]
    G = [A.alloc([128, 4, 256], F32, "G%d" % i) for i in range(2)]
    rt = [A.alloc([128, 512], F32, "rt%d" % i) for i in range(4)]
    PTr = [A.alloc([128, 128], BF16, "PTr%d" % i) for i in range(2)]
    oall = A.alloc([128, 8, 256], F32, "oall")
    bst = A.alloc([128, 8, 6], F32, "bst")
    mv = A.alloc([128, 8, 2], F32, "mv")
    rstd_r = A.alloc([128, 8], F32, "rstd_r")
    ynorm = [A.alloc([128, 256], F32, "ynorm%d" % i) for i in range(2)]

    S.op('sp', lambda e: e.dma_start(out=ropet[:], in_=ropet_d.ap()), w=['ropet'], kind='d')
    S.op('sp', lambda e: e.dma_start(out=rtab[:], in_=rtab_d.ap()), w=['rtab'], kind='d')
    S.op('sp', lambda e: e.dma_start(out=dmk[:], in_=dmk_d.ap()), w=['dmk'], kind='d')
    S.op('sp', lambda e: e.dma_start(out=decb[:], in_=dec.partition_broadcast(128)), w=['decb'], kind='d')
    S.op('sp', lambda e: e.dma_start(out=gnb[:], in_=gnw.partition_broadcast(128)), w=['gnb'], kind='d')
    S.op('act', lambda e: e.activation(out=lg[:], in_=decb[:], func=AF.Exp, scale=-1.0), r=['decb'], w=['lg'])
    S.op('act', lambda e: e.activation(out=lg[:], in_=lg[:], func=AF.Ln, bias=1.0), r=['lg'], w=['lg'])
    S.op('dve', lambda e: e.tensor_scalar(out=lg[:], in0=lg[:], scalar1=-1.0, scalar2=None, op0=ALU.mult),
         r=['lg'], w=['lg'])
    for h in range(4):
        S.op('act', lambda e, h=h: e.activation(out=dc[:, h, 0:10], in_=rtab[:, 0:10], func=AF.Exp,
                                                scale=lg[:, h:h + 1]), r=['lg', 'rtab'], w=[('dc', h)])
        S.op('act', lambda e, h=h: e.activation(out=dc[:, h, 10:20], in_=rtab[:, 10:20], func=AF.Exp,
                                                scale=lg[:, 4 + h:5 + h]), r=['lg', 'rtab'], w=[('dc', h)])
        S.op('dve', lambda e, h=h: e.tensor_tensor(out=dc[:, h, 5:10], in0=dc[:, h, 5:10], in1=rtab[:, 20:25],
                                                   op=ALU.mult), r=[('dc', h), 'rtab'], w=[('dc', h)])
        S.op('dve', lambda e, h=h: e.tensor_tensor(out=dc[:, h, 15:20], in0=dc[:, h, 15:20], in1=rtab[:, 25:30],
                                                   op=ALU.mult), r=[('dc', h), 'rtab'], w=[('dc', h)])
        S.op('dve', lambda e, h=h: e.tensor_scalar(out=dc[:, h, 1:2], in0=dc[:, h, 1:2], scalar1=1.0 / 16,
                                                   scalar2=None, op0=ALU.mult), r=[('dc', h)], w=[('dc', h)])
        S.op('dve', lambda e, h=h: e.tensor_scalar(out=dc[:, h, 11:12], in0=dc[:, h, 11:12], scalar1=1.0 / 16,
                                                   scalar2=None, op0=ALU.mult), r=[('dc', h)], w=[('dc', h)])

    if stop == 'E0':
        dump('dc', dc[:], [128, 4, 20], [('dc', h_) for h_ in range(4)])
        return finish()
    own0 = 0
    for h in range(4):
        dck = ('dc', h)
        cols = [h * 256, 1024 + h * 256, 2048 + h * 256, 3072 + h * 256]
        for i in (1, 2, 0, 3):
            S.op('pool', lambda e, i=i, c=cols[i]: e.dma_start(
                out=ring[i][:], in_=win[:, c:c + 256].rearrange("(k p) n -> p k n", p=128)),
                w=[('ring', i, 0), ('ring', i, 1)], kind='d')
        def blk_dfb():
            S.op('act', lambda e, h=h: e.activation(out=e12[:, 0, :], in_=dmk[:, 0, :], func=AF.Exp, scale=lg[:, h:h + 1]),
                 r=['lg', 'dmk'], w=['e12'])
            S.op('act', lambda e, h=h: e.activation(out=e12[:, 1, :], in_=dmk[:, 1, :], func=AF.Exp,
                                                    scale=lg[:, 4 + h:5 + h]), r=['lg', 'dmk'], w=['e12'])
            S.op('dve', lambda e: e.tensor_tensor(out=e12[:], in0=e12[:], in1=dmk[:, 2:4, :], op=ALU.mult),
                 r=['e12', 'dmk'], w=['e12'])
            S.op('dve', lambda e: e.tensor_tensor(out=Dfb[:], in0=e12[:, 0, :], in1=e12[:, 1, :], op=ALU.add),
                 r=['e12'], w=['Dfb'])
        def blk_qk(dst, dkey, slot):
            for half in range(2):
                ba = 2 * half
                for blk in range(2):
                    for k in range(16):
                        S.op('pe', lambda e, k=k, blk=blk, half=half, slot=slot, ba=ba: e.matmul(
                            psb(ba + blk), lhsT=ring[slot][:, k, blk * 128:(blk + 1) * 128],
                            rhs=hTa[:, k, own0 + half * 512:own0 + (half + 1) * 512], start=(k == 0), stop=(k == 15)),
                            r=[('ring', slot, blk)] + hT_own, w=[pskey(ba + blk)])
                cs = ropet[:, 0, half * 512:(half + 1) * 512]
                sn = ropet[:, 1, half * 512:(half + 1) * 512]
                pa, pb = psb(ba), psb(ba + 1)
                S.op('dve', lambda e, pa=pa, cs=cs: e.tensor_tensor(out=rt[0][:], in0=pa, in1=cs, op=ALU.mult),
                     r=[pskey(ba), 'ropet'], w=[('rt', 0)])
                S.op('dve', lambda e, pb=pb, sn=sn: e.tensor_tensor(out=rt[1][:], in0=pb, in1=sn, op=ALU.mult),
                     r=[pskey(ba + 1), 'ropet'], w=[('rt', 1)])
                S.op('dve', lambda e, pa=pa, sn=sn: e.tensor_tensor(out=rt[2][:], in0=pa, in1=sn, op=ALU.mult),
                     r=[pskey(ba), 'ropet'], w=[('rt', 2)])
                S.op('dve', lambda e, pb=pb, cs=cs: e.tensor_tensor(out=rt[3][:], in0=pb, in1=cs, op=ALU.mult),
                     r=[pskey(ba + 1), 'ropet'], w=[('rt', 3)])
                S.op('dve', lambda e, dst=dst, half=half: e.tensor_sub(out=dst[:, 0, half * 512:(half + 1) * 512],
                                                                         in0=rt[0][:], in1=rt[1][:]),
                     r=[('rt', 0), ('rt', 1)], w=[(dkey, half)])
                S.op('dve', lambda e, dst=dst, half=half: e.tensor_add(out=dst[:, 1, half * 512:(half + 1) * 512],
                                                                         in0=rt[2][:], in1=rt[3][:]),
                     r=[('rt', 2), ('rt', 3)], w=[(dkey, half)])
        qk_keys = [('qT', 0), ('qT', 1), ('kT', 0), ('kT', 1)]
        def blk_ktm():
            zcol = small[:, 8:9]
            if h == 0:
                S.op('pool', lambda e: e.memset(small[:, 8:16], 0.0), w=['zcol'])
            for c in range(8):
                bank = 4 + c % 2
                pstv = PS[:, bank:bank + 1, :].rearrange("p b f -> p (b f)").bitcast(BF16).rearrange("p (k t) -> p k t", t=128)
                for blk in range(2):
                    S.op('pe', lambda e, c=c, blk=blk, pstv=pstv: e.transpose(pstv[:, blk, :],
                                                                             kT[:, blk, c * 128:(c + 1) * 128], ident[:]),
                         r=[('kT', c // 4), 'ident'], w=[pskey(bank)])
                for blk in range(2):
                    S.op('act', lambda e, c=c, blk=blk, pstv=pstv, h=h: e.activation(
                        out=ktm[0][:, c, blk * 128:(blk + 1) * 128], in_=pstv[:, blk, :], func=AF.Identity,
                        scale=dc[:, h, 0:1], bias=zcol), r=[pskey(bank), dck, 'zcol'], w=[('ktm', 0, c)])
                    S.op('dve', lambda e, c=c, blk=blk, pstv=pstv, h=h: e.tensor_scalar(
                        out=ktm[1][:, c, blk * 128:(blk + 1) * 128], in0=pstv[:, blk, :], scalar1=dc[:, h, 10:11],
                        scalar2=zcol, op0=ALU.mult, op1=ALU.add), r=[pskey(bank), dck, 'zcol'], w=[('ktm', 1, c)])
        def blk_vg(which_list):
            for c in range(8):
                for which, (slot, dstt, dk) in [(w_, [(2, vtm, 'vtm'), (3, gs, 'gs')][w_]) for w_ in which_list]:
                    bank = 6 + c % 2
                    for k in range(16):
                        S.op('pe', lambda e, k=k, c=c, slot=slot, bank=bank: e.matmul(
                            psb(bank)[:, 0:256], lhsT=hTa[:, k, own0 + c * 128:own0 + (c + 1) * 128], rhs=ring[slot][:, k, :],
                            start=(k == 0), stop=(k == 15)),
                            r=[('ring', slot, 0), ('ring', slot, 1), ('hT', 2 + c)], w=[pskey(bank)])
                    S.op('act', lambda e, c=c, dstt=dstt, bank=bank, which=which: e.activation(
                        out=dstt[:, c, :], in_=psb(bank)[:, 0:256], func=(AF.Copy if which == 0 else AF.Silu)),
                        r=[pskey(bank)], w=[(dk, c)])
        def blk_ctx():
            for t in range(2):
                for k in range(16):
                    S.op('pe', lambda e, k=k, t=t: e.matmul(
                        psb(4)[:, 0:256], lhsT=hTa[:, k, 1024 + t * 128:1024 + (t + 1) * 128], rhs=ring[1][:, k, :],
                        start=(k == 0), stop=(k == 15)),
                        r=[('ring', 1, 0), ('ring', 1, 1), ('hT', 12 + t)], w=[pskey(4)])
                S.op('act', lambda e, t=t, h=h: e.activation(out=cktm[0][:, t, :], in_=psb(4)[:, 0:256], func=AF.Copy,
                                                            scale=dc[:, h, 3 + t:4 + t]),
                     r=[pskey(4), dck], w=[('cktm', 0)])
                S.op('dve', lambda e, t=t, h=h: e.tensor_scalar(out=cktm[1][:, t, :], in0=psb(4)[:, 0:256],
                                                               scalar1=dc[:, h, 13 + t:14 + t], scalar2=None,
                                                               op0=ALU.mult),
                     r=[pskey(4), dck], w=[('cktm', 1)])
                for k in range(16):
                    S.op('pe', lambda e, k=k, t=t: e.matmul(
                        psb(5)[:, 0:256], lhsT=hTa[:, k, 1024 + t * 128:1024 + (t + 1) * 128], rhs=ring[2][:, k, :],
                        start=(k == 0), stop=(k == 15)),
                        r=[('ring', 2, 0), ('ring', 2, 1), ('hT', 12 + t)], w=[pskey(5)])
                S.op('act', lambda e, t=t: e.activation(out=cvtm[:, t, :], in_=psb(5)[:, 0:256], func=AF.Copy),
                     r=[pskey(5)], w=['cvtm'])
        ktm_keys = [[('ktm', d, c) for c in range(8)] for d in range(2)]
        vtm_keys = [('vtm', c) for c in range(8)]

        def recur(store):
            for d in range(2):
                order = range(8) if d == 0 else range(7, -1, -1)
                cd = dc[:, h, 2:3] if d == 0 else dc[:, h, 12:13]
                for c in order:
                    if store:
                        S.op('act', lambda e, d=d, c=c: e.activation(
                            out=Sbf[d][:, c, :, :].rearrange("p b v -> p (b v)"),
                            in_=St[d][:].rearrange("p b v -> p (b v)"), func=AF.Copy),
                            r=[('St', d)], w=[('Sbf', d, c)])
                    bank = c % 2
                    pm = psb(bank).rearrange("p (b v) -> p b v", v=256)
                    for blk in range(2):
                        S.op('pe', lambda e, d=d, c=c, blk=blk, pm=pm: e.matmul(
                            pm[:, blk, :], lhsT=ktm[d][:, c, blk * 128:(blk + 1) * 128], rhs=vtm[:, c, :],
                            start=True, stop=True), r=[('ktm', d, c), ('vtm', c)], w=[pskey(bank)])
                    S.op('dve', lambda e, d=d, cd=cd, bank=bank: e.scalar_tensor_tensor(
                        out=St[d][:].rearrange("p b v -> p (b v)"), in0=St[d][:].rearrange("p b v -> p (b v)"),
                        scalar=cd, in1=psb(bank), op0=ALU.mult, op1=ALU.add),
                        r=[('St', d), pskey(bank), dck], w=[('St', d)])

        blk_qk(kT, 'kT', 1)
        blk_ktm()
        blk_vg([0])
        blk_ctx()
        for d in range(2):
            S.op('pool', lambda e, d=d: e.memset(St[d][:], 0.0), w=[('St', d)])
        recur(False)
        for d in range(2):
            S.op('sp', lambda e, d=d, h=h: e.dma_start(
                out=stin[h].ap().rearrange("(a p) v -> p a v", p=128)[:, 2 * d:2 * d + 2, :], in_=St[d][:]),
                r=[('St', d)], w=[('stin', h)], kind='d')
        S.op('pool', lambda e, h=h: e.collective_compute(
            "AllGather", ALU.bypass, replica_groups=[[0, 1, 2, 3], [4, 5, 6, 7]],
            ins=[stin[h].ap().opt()], outs=[stall[h].ap().opt()]), r=[('stin', h)], w=[('stall', h)], kind='cc')
        blk_dfb()
        blk_qk(qT, 'qT', 0)
        blk_vg([1])
        for d in range(2):
            pc = psb(2 + d).rearrange("p (b v) -> p b v", v=256)
            for blk in range(2):
                for t in range(2):
                    S.op('pe', lambda e, d=d, blk=blk, t=t, pc=pc: e.matmul(
                        pc[:, blk, :], lhsT=cktm[d][:, t, blk * 128:(blk + 1) * 128], rhs=cvtm[:, t, :],
                        start=(t == 0), stop=(t == 1)), r=[('cktm', d), 'cvtm'], w=[pskey(2 + d)])
            c0 = 5 if d == 0 else 15
            S.op('dve', lambda e, d=d, c0=c0, h=h: e.tensor_scalar(
                out=St[d][:].rearrange("p b v -> p (b v)"), in0=psb(2 + d), scalar1=dc[:, h, c0:c0 + 1], scalar2=None,
                op0=ALU.mult), r=[pskey(2 + d), dck], w=[('St', d)])
        for r_ in range(4):
            S.op('sp', lambda e, h=h, r_=r_: e.dma_start(
                out=G[r_ % 2][:], in_=stall[h].ap()[r_ * 512:(r_ + 1) * 512, :].rearrange("(a p) v -> p a v", p=128)),
                r=[('stall', h)], w=[('G', r_ % 2)], kind='d')
            for d in range(2):
                c0 = 5 if d == 0 else 15
                S.op('dve', lambda e, d=d, r_=r_, c0=c0, h=h: e.scalar_tensor_tensor(
                    out=St[d][:], in0=G[r_ % 2][:, 2 * d:2 * d + 2, :], scalar=dc[:, h, c0 + 1 + r_:c0 + 2 + r_],
                    in1=St[d][:], op0=ALU.mult, op1=ALU.add), r=[('G', r_ % 2), ('St', d), dck], w=[('St', d)])
        if stop == 'E3':
            dump('St0', St[0][:], [128, 2, 256], [('St', 0)])
            dump('St1', St[1][:], [128, 2, 256], [('St', 1)])
            return finish()
        recur(True)
        def p2b_scores(c):
            p2 = c % 2
            pS = psb(4 + p2)[:, 0:128]
            for blk in range(2):
                S.op('pe', lambda e, blk=blk: e.matmul(
                    pS, lhsT=kT[:, blk, c * 128:(c + 1) * 128], rhs=qT[:, blk, c * 128:(c + 1) * 128],
                    start=(blk == 0), stop=(blk == 1)), r=qk_keys, w=[pskey(4 + p2)])
            S.op('dve', lambda e: e.tensor_tensor(out=PTr[p2][:], in0=pS, in1=Dfb[:], op=ALU.mult),
                 r=[pskey(4 + p2), 'Dfb'], w=[('PTr', p2)])

        p2b_scores(0)
        for c in range(8):
            p2 = c % 2
            if c + 1 < 8:
                p2b_scores(c + 1)
            pI = psb(6 + p2)[:, 0:256]
            pF = psb(6 + p2)[:, 256:512]
            pB = psb(p2)[:, 0:256]
            for blk in range(2):
                S.op('pe', lambda e, c=c, blk=blk, pF=pF: e.matmul(
                    pF, lhsT=qT[:, blk, c * 128:(c + 1) * 128], rhs=Sbf[0][:, c, blk, :], start=(blk == 0),
                    stop=(blk == 1)), r=qk_keys + [('Sbf', 0, c)], w=[pskey(6 + p2)])
            for blk in range(2):
                S.op('pe', lambda e, c=c, blk=blk, pB=pB: e.matmul(
                    pB, lhsT=qT[:, blk, c * 128:(c + 1) * 128], rhs=Sbf[1][:, c, blk, :], start=(blk == 0),
                    stop=(blk == 1)), r=qk_keys + [('Sbf', 1, c)], w=[pskey(p2)])
            S.op('pe', lambda e, c=c, p2=p2, pI=pI: e.matmul(pI, lhsT=PTr[p2][:], rhs=vtm[:, c, :], start=True,
                                                            stop=True),
                 r=[('PTr', p2), ('vtm', c)], w=[pskey(6 + p2)])
            S.op('act', lambda e, c=c, pI=pI: e.activation(out=oall[:, c, :], in_=pI, func=AF.Copy),
                 r=[pskey(6 + p2)], w=[('oall', c)])
            S.op('dve', lambda e, c=c, pF=pF, h=h: e.scalar_tensor_tensor(
                out=oall[:, c, :], in0=pF, scalar=dc[:, h, 1:2], in1=oall[:, c, :], op0=ALU.mult, op1=ALU.add),
                r=[pskey(6 + p2), ('oall', c), dck], w=[('oall', c)])
            S.op('dve', lambda e, c=c, pB=pB, h=h: e.scalar_tensor_tensor(
                out=oall[:, c, :], in0=pB, scalar=dc[:, h, 11:12], in1=oall[:, c, :], op0=ALU.mult, op1=ALU.add),
                r=[pskey(p2), ('oall', c), dck], w=[('oall', c)])
            S.op('dve', lambda e, c=c: e.bn_stats(out=bst[:, c, :], in_=oall[:, c, :]), r=[('oall', c)], w=[('bst', c)])
            S.op('dve', lambda e, c=c: e.bn_aggr(out=mv[:, c, :], in_=bst[:, c, :]), r=[('bst', c)], w=['mv'])
        if stop == 'E4':
            dump('oall', oall[:], [128, 8, 256], [('oall', c_) for c_ in range(8)])
            return finish()
        S.op('dve', lambda e: e.tensor_scalar(out=rstd_r[:], in0=mv[:, :, 1], scalar1=EPS, scalar2=None, op0=ALU.add),
             r=['mv'], w=['rstd_r'])
        S.op('act', lambda e: e.activation(out=rstd_r[:], in_=rstd_r[:], func=AF.Sqrt), r=['rstd_r'], w=['rstd_r'])
        S.op('dve', lambda e: e.reciprocal(out=rstd_r[:], in_=rstd_r[:]), r=['rstd_r'], w=['rstd_r'])
        for c in range(8):
            p2 = c % 2
            S.op('dve', lambda e, c=c, p2=p2: e.tensor_scalar(
                out=ynorm[p2][:], in0=oall[:, c, :], scalar1=mv[:, c, 0:1], scalar2=rstd_r[:, c:c + 1],
                op0=ALU.subtract, op1=ALU.mult), r=[('oall', c), 'mv', 'rstd_r'], w=[('yn', p2)])
            S.op('dve', lambda e, p2=p2, h=h: e.tensor_mul(out=ynorm[p2][:], in0=ynorm[p2][:],
                                                            in1=gnb[:, h * 256:(h + 1) * 256]),
                 r=[('yn', p2), 'gnb'], w=[('yn', p2)])
            S.op('dve', lambda e, c=c, p2=p2, h=h: e.tensor_mul(out=cat[:, c, h * 256:(h + 1) * 256],
                                                                 in0=ynorm[p2][:], in1=gs[:, c, :]),
                 r=[('yn', p2), ('gs', c)], w=[('cat', c)])

    dump('cat2', cat[:], [128, 8, 2048], [('cat', m) for m in range(8)], BF16)
    if stop == 'E':
        return finish()
    S.bar()

    A.seek(159, 207.5)
    gbc = A.alloc([128, 2, 2048], F32, "gbc")
    hfT = A.alloc([128, 16, 1024], BF16, "hfT")
    A.seek(38, 159)
    woutb = A.alloc([128, 16, 2048], BF16, "woutb")
    gB = A.alloc([128, 16, 128], F32, "gB")
    xr = [A.alloc([128, 2048], F32, "xr%d" % i) for i in range(2)]
    x1t = [A.alloc([128, 2048], F32, "x1t%d" % i) for i in range(2)]
    catT = [A.alloc([128, 16, 128], BF16, "catT%d" % i) for i in range(2)]
    xn2 = [A.alloc([128, 2048], BF16, "xn2%d" % i) for i in range(2)]

    for j in range(4):
        S.op('pool', lambda e, j=j: e.dma_start(
            out=woutb[:, :, j * 512:(j + 1) * 512],
            in_=wout[:, j * 512:(j + 1) * 512].rearrange("(k p) n -> p k n", p=128)), w=[('woutb', j)], kind='d')
    S.op('pool', lambda e: e.dma_start(out=wrb[:], in_=wrt.rearrange("(k p) n -> p k n", p=128)), w=['wrb'], kind='d')
    S.op('sp', lambda e: e.dma_start(out=brb[:], in_=brt.partition_broadcast(128)), w=['brb'], kind='d')
    for gi, base in enumerate([32, 80]):
        S.op('dve', lambda e, base=base: e.tensor_copy(
            out=gB[:], in_=modc[:, base:base + 16, 0:1].to_broadcast([128, 16, 128])),
            r=[('modc', 1, 0)], w=['gB'])
        for q4 in range(4):
            for kk in range(4):
                k = q4 * 4 + kk
                S.op('pe', lambda e, k=k, kk=kk: e.matmul(psb(0)[:, kk * 128:(kk + 1) * 128], lhsT=gB[:, k, :],
                                                        rhs=identf[:], start=True, stop=True),
                     r=['gB', 'identf'], w=[pskey(0)])
            S.op('act', lambda e, gi=gi, q4=q4: e.activation(out=gbc[:, gi, q4 * 512:(q4 + 1) * 512], in_=psb(0),
                                                            func=AF.Copy), r=[pskey(0)], w=[('gbc', gi)])
    S.op('pool', lambda e: e.memset(rsm[:], 0.0), w=['rsm'])

    def f_stage1(i):
        p2 = i % 2
        S.op('sp', lambda e, i=i, p2=p2: e.dma_start(out=xr[p2][:], in_=xs[256 + i * 128:256 + (i + 1) * 128, :]),
             w=[('xr', p2)], kind='d')
        pst = PS[:, 2:4, :].rearrange("p b f -> p (b f)").bitcast(BF16).rearrange("p (k t) -> p k t", t=128)
        for k in range(16):
            S.op('pe', lambda e, k=k, i=i, pst=pst: e.transpose(pst[:, k, :], cat[:, i, k * 128:(k + 1) * 128], ident[:]),
                 r=[('cat', i), 'ident'], w=[pskey(2 + k // 8)])
        S.op('dve', lambda e, p2=p2, pst=pst: e.tensor_copy(out=catT[p2][:, 0:8, :], in_=pst[:, 0:8, :]),
             r=[pskey(2)], w=[('catT', p2, 0)])
        S.op('act', lambda e, p2=p2, pst=pst: e.activation(out=catT[p2][:, 8:16, :], in_=pst[:, 8:16, :], func=AF.Copy),
             r=[pskey(3)], w=[('catT', p2, 1)])
        for j in range(4):
            bank = 4 + j % 2
            for k in range(16):
                S.op('pe', lambda e, k=k, j=j, p2=p2, bank=bank: e.matmul(
                    psb(bank), lhsT=catT[p2][:, k, :], rhs=woutb[:, k, j * 512:(j + 1) * 512], start=(k == 0),
                    stop=(k == 15)), r=[('catT', p2, k // 8), ('woutb', j)], w=[pskey(bank)])
            S.op('dve', lambda e, j=j, p2=p2, bank=bank: e.tensor_tensor(
                out=x1t[p2][:, j * 512:(j + 1) * 512], in0=psb(bank), in1=gbc[:, 0, j * 512:(j + 1) * 512], op=ALU.mult),
                r=[pskey(bank), ('gbc', 0)], w=[('x1t', p2, j)])
            S.op('dve', lambda e, j=j, p2=p2: e.tensor_add(
                out=x1t[p2][:, j * 512:(j + 1) * 512], in0=x1t[p2][:, j * 512:(j + 1) * 512],
                in1=xr[p2][:, j * 512:(j + 1) * 512]), r=[('x1t', p2, j), ('xr', p2)], w=[('x1t', p2, j)])
        x1keys = [('x1t', p2, j) for j in range(4)]
        S.op('pool', lambda e, i=i, p2=p2: e.dma_start(out=x1s[i * 128:(i + 1) * 128, :], in_=x1t[p2][:]),
             r=x1keys, w=[('x1s', i)], kind='d')
        sc_, rc_ = rsm[:, i:i + 1], rsm[:, 8 + i:9 + i]
        S.op('act', lambda e, p2=p2, sc_=sc_: e.activation(out=xn2[p2][:], in_=x1t[p2][:], func=AF.Square, accum_out=sc_),
             r=x1keys + ['rsm'], w=[('xn2', p2), ('F', i, 'ss')])
        S.op('dve', lambda e, sc_=sc_, rc_=rc_: e.tensor_scalar(out=rc_, in0=sc_, scalar1=1.0 / 2048, scalar2=EPS,
                                                               op0=ALU.mult, op1=ALU.add),
             r=[('F', i, 'ss')], w=[('F', i, 'rs')])
        S.op('act', lambda e, rc_=rc_: e.activation(out=rc_, in_=rc_, func=AF.Sqrt), r=[('F', i, 'rs')],
             w=[('F', i, 'rs')])
        S.op('dve', lambda e, rc_=rc_: e.reciprocal(out=rc_, in_=rc_), r=[('F', i, 'rs')], w=[('F', i, 'rs')])
        S.op('dve', lambda e, p2=p2, rc_=rc_: e.tensor_scalar(out=xn2[p2][:], in0=x1t[p2][:], scalar1=rc_, scalar2=None,
                                                              op0=ALU.mult), r=x1keys + [('F', i, 'rs')],
             w=[('xn2', p2)])
    def f_stage2(i):
        p2 = i % 2
        transpose_mod(xn2[p2], ('xn2', p2),
                      lambda k, i=i: hfT[:, k, i * 128:(i + 1) * 128],
                      lambda k, i=i: ('hfT', i),
                      lambda k: ABc[:, 2, k:k + 1],
                      lambda k: modc[:, 48 + k, 0:1],
                      'A2', ('modc', 1, 0), 6)
        for k in range(16):
            S.op('pe', lambda e, k=k, i=i: e.matmul(psb(1)[:, 0:36], lhsT=hfT[:, k, i * 128:(i + 1) * 128],
                                                   rhs=wrb[:, k, :], start=(k == 0), stop=(k == 15)),
                 r=[('hfT', i), 'wrb'], w=[pskey(1)])
        R = 'rtr'

        def dv(fn, r, w):
            S.op('dve', fn, r=[(R, x) for x in r], w=[(R, x) for x in w])
        sm = rsm[:, 16:64]
        gmax, ngmax, gsum, gw = sm[:, 0:1], sm[:, 1:2], sm[:, 2:3], sm[:, 3:4]
        goh, gex = sm[:, 4:8], sm[:, 8:12]
        esel, mx8 = sm[:, 12:20], sm[:, 20:28]
        mk1, mk2 = sm[:, 28:36], sm[:, 36:44]
        dd, ee, w1, w2 = sm[:, 44:45], sm[:, 45:46], sm[:, 46:47], sm[:, 47:48]
        S.op('dve', lambda e: e.tensor_tensor(out=lgt[:], in0=psb(1)[:, 0:36], in1=brb[:], op=ALU.add),
             r=[pskey(1), 'brb'], w=[(R, 'lgt')])
        dv(lambda e: e.reduce_max(out=gmax, in_=lgt[:, 0:4], axis=AX.X), ['lgt'], ['gmax'])
        dv(lambda e: e.tensor_scalar(out=goh, in0=lgt[:, 0:4], scalar1=gmax, scalar2=None, op0=ALU.is_equal),
           ['lgt', 'gmax'], ['goh'])
        dv(lambda e: e.tensor_scalar(out=ngmax, in0=gmax, scalar1=-1.0, scalar2=None, op0=ALU.mult), ['gmax'], ['ngmax'])
        dv(lambda e: e.memset(gsum, 0.0), [], ['gsum'])
        S.op('act', lambda e: e.activation(out=gex, in_=lgt[:, 0:4], func=AF.Exp, bias=ngmax, accum_out=gsum),
             r=[(R, 'lgt'), (R, 'ngmax'), (R, 'gsum')], w=[(R, 'gsum'), (R, 'gex')])
        dv(lambda e: e.reciprocal(out=gw, in_=gsum), ['gsum'], ['gw'])
        dv(lambda e: e.tensor_scalar(out=esel, in0=lgt[:, 4:12], scalar1=goh[:, 0:1], scalar2=None, op0=ALU.mult),
           ['lgt', 'goh'], ['esel'])
        for g in range(1, 4):
            dv(lambda e, g=g: e.scalar_tensor_tensor(out=esel, in0=lgt[:, 4 + 8 * g:12 + 8 * g], scalar=goh[:, g:g + 1],
                                                     in1=esel, op0=ALU.mult, op1=ALU.add), ['lgt', 'goh', 'esel'],
               ['esel'])
        dv(lambda e: e.max(out=mx8, in_=esel), ['esel'], ['mx8'])
        dv(lambda e: e.tensor_scalar(out=mk1, in0=esel, scalar1=mx8[:, 0:1], scalar2=None, op0=ALU.is_equal),
           ['esel', 'mx8'], ['mk1'])
        dv(lambda e: e.tensor_scalar(out=mk2, in0=esel, scalar1=mx8[:, 1:2], scalar2=None, op0=ALU.is_equal),
           ['esel', 'mx8'], ['mk2'])
        dv(lambda e: e.tensor_sub(out=dd, in0=mx8[:, 1:2], in1=mx8[:, 0:1]), ['mx8'], ['dd'])
        S.op('act', lambda e: e.activation(out=ee, in_=dd, func=AF.Exp), r=[(R, 'dd')], w=[(R, 'ee')])
        dv(lambda e: e.tensor_scalar(out=w1, in0=ee, scalar1=1.0, scalar2=None, op0=ALU.add), ['ee'], ['w1'])
        dv(lambda e: e.reciprocal(out=w1, in_=w1), ['w1'], ['w1'])
        dv(lambda e: e.tensor_mul(out=w2, in0=ee, in1=w1), ['ee', 'w1'], ['w2'])
        dv(lambda e: e.tensor_mul(out=w1, in0=w1, in1=gw), ['w1', 'gw'], ['w1'])
        dv(lambda e: e.tensor_mul(out=w2, in0=w2, in1=gw), ['w2', 'gw'], ['w2'])
        dv(lambda e: e.tensor_scalar(out=mk1, in0=mk1, scalar1=w1, scalar2=None, op0=ALU.mult), ['mk1', 'w1'], ['mk1'])
        dv(lambda e: e.scalar_tensor_tensor(out=mk1, in0=mk2, scalar=w2, in1=mk1, op0=ALU.mult, op1=ALU.add),
           ['mk1', 'mk2', 'w2'], ['mk1'])
        for g in range(4):
            S.op('dve', lambda e, g=g, i=i: e.tensor_scalar(out=comb[:, i, 8 * g:8 * g + 8], in0=mk1,
                                                           scalar1=goh[:, g:g + 1], scalar2=None, op0=ALU.mult),
                 r=[(R, 'mk1'), (R, 'goh')], w=[('comb', i)])
    for i in range(9):
        if i < 8:
            f_stage1(i)
        if i >= 1:
            f_stage2(i - 1)
    dump('hfT', hfT[:], [128, 16, 1024], [('hfT', i) for i in range(8)], BF16)
    dump('comb', comb[:], [128, 8, 32], [('comb', i) for i in range(8)])
    dump('gbc', gbc[:], [128, 2, 2048], [('gbc', 0), ('gbc', 1)])
    if stop == 'F':
        return finish()
    S.bar()
    A.seek(6, 159)

    acc = A.alloc([128, 8, 2048], F32, "acc")
    wring = [A.alloc([128, 8192], BF16, "wring%d" % i) for i in range(4)]
    aT = [A.alloc([128, 4, 1024], BF16, "aT%d" % i) for i in range(2)]
    sg = [A.alloc([128, 512], F32, "sg%d" % i) for i in range(2)]
    hf_keys = [('hfT', i) for i in range(8)]
    wi = [0]

    def wslot():
        s = wi[0] % 4
        wi[0] += 1
        return s
    pcnt = [0]
    for ex in range(32):
        sg_, su_, sd_ = wslot(), wslot(), wslot()
        Wg = wring[sg_][:].rearrange("p (k n) -> p k n", n=512)
        Wu = wring[su_][:].rearrange("p (k n) -> p k n", n=512)
        Wd = wring[sd_][:].rearrange("p (k n) -> p k n", n=2048)
        S.op('pool', lambda e, Wg=Wg, ex=ex: e.dma_start(out=Wg, in_=wg[ex].rearrange("(k p) n -> p k n", p=128)),
             w=[('wr', sg_)], kind='d')
        S.op('pool', lambda e, Wu=Wu, ex=ex: e.dma_start(out=Wu, in_=wu[ex].rearrange("(k p) n -> p k n", p=128)),
             w=[('wr', su_)], kind='d')
        S.op('pool', lambda e, Wd=Wd, ex=ex: e.dma_start(out=Wd, in_=wd[ex].rearrange("(k p) n -> p k n", p=128)),
             w=[('wr', sd_)], kind='d')
        ab = ex % 2
        for f in range(4):
            for th in range(2):
                bg = (pcnt[0] % 2) * 2
                pcnt[0] += 1
                for wi_, (W_, sk) in enumerate([(Wg, sg_), (Wu, su_)]):
                    for k in range(16):
                        S.op('pe', lambda e, W_=W_, k=k, f=f, th=th, bg=bg, wi_=wi_: e.matmul(
                            psb(bg + wi_), lhsT=W_[:, k, f * 128:(f + 1) * 128], rhs=hfT[:, k, th * 512:(th + 1) * 512],
                            start=(k == 0), stop=(k == 15)), r=[('wr', sk)] + hf_keys[th * 4:th * 4 + 4],
                            w=[pskey(bg + wi_)])
                sgi = pcnt[0] % 2
                S.op('act', lambda e, bg=bg, sgi=sgi: e.activation(out=sg[sgi][:], in_=psb(bg), func=AF.Silu),
                     r=[pskey(bg)], w=[('sg', sgi)])
                S.op('dve', lambda e, bg=bg, sgi=sgi, f=f, th=th, ab=ab: e.tensor_tensor(
                    out=aT[ab][:, f, th * 512:(th + 1) * 512], in0=psb(bg + 1), in1=sg[sgi][:], op=ALU.mult),
                    r=[pskey(bg + 1), ('sg', sgi)], w=[('aT', ab, th)])
        for i in range(8):
            for j in range(4):
                bank = 4 + (i * 4 + j) % 4
                for k in range(4):
                    S.op('pe', lambda e, k=k, i=i, j=j, bank=bank, ab=ab, Wd=Wd: e.matmul(
                        psb(bank), lhsT=aT[ab][:, k, i * 128:(i + 1) * 128], rhs=Wd[:, k, j * 512:(j + 1) * 512],
                        start=(k == 0), stop=(k == 3)), r=[('aT', ab, i // 4), ('wr', sd_)], w=[pskey(bank)])
                if ex == 0:
                    S.op('dve', lambda e, i=i, j=j, bank=bank, ex=ex: e.tensor_scalar(
                        out=acc[:, i, j * 512:(j + 1) * 512], in0=psb(bank), scalar1=comb[:, i, ex:ex + 1], scalar2=None,
                        op0=ALU.mult), r=[pskey(bank), ('comb', i)], w=[('acc', i, j)])
                else:
                    S.op('dve', lambda e, i=i, j=j, bank=bank, ex=ex: e.scalar_tensor_tensor(
                        out=acc[:, i, j * 512:(j + 1) * 512], in0=psb(bank), scalar=comb[:, i, ex:ex + 1],
                        in1=acc[:, i, j * 512:(j + 1) * 512], op0=ALU.mult, op1=ALU.add),
                        r=[pskey(bank), ('comb', i), ('acc', i, j)], w=[('acc', i, j)])

    S.bar()
    A.seek(6 + 64, 159)
    x1r = [A.alloc([128, 2048], F32, "x1r%d" % i) for i in range(2)]
    yo = [A.alloc([128, 2048], F32, "yo%d" % i) for i in range(2)]
    w3b = A.alloc([128, 2048], F32, "w3b")
    junk3 = A.alloc([128, 2048], BF16, "junk3")
    S.op('sp', lambda e: e.dma_start(out=w3b[:], in_=fnw.partition_broadcast(128)), w=['w3b'], kind='d')
    S.op('pool', lambda e: e.memset(rsm[:, 0:16], 0.0), w=['rsm2'])
    outs = []
    for i in range(8):
        p2 = i % 2
        S.op('sp', lambda e, i=i, p2=p2: e.dma_start(out=x1r[p2][:], in_=x1s[i * 128:(i + 1) * 128, :]),
             r=[('x1s', i)], w=[('x1r', p2)], kind='d')
        acck = [('acc', i, j) for j in range(4)]
        S.op('dve', lambda e, i=i, p2=p2: e.tensor_tensor(out=yo[p2][:], in0=acc[:, i, :], in1=gbc[:, 1, :], op=ALU.mult),
             r=acck + [('gbc', 1)], w=[('yo', p2)])
        S.op('dve', lambda e, p2=p2: e.tensor_add(out=yo[p2][:], in0=yo[p2][:], in1=x1r[p2][:]),
             r=[('yo', p2), ('x1r', p2)], w=[('yo', p2)])
        sc_, rc_ = rsm[:, i:i + 1], rsm[:, 8 + i:9 + i]
        S.op('act', lambda e, p2=p2, sc_=sc_: e.activation(out=junk3[:], in_=yo[p2][:], func=AF.Square, accum_out=sc_),
             r=[('yo', p2), 'rsm2'], w=['junk3', ('I', i, 'ss')])
        S.op('dve', lambda e, sc_=sc_, rc_=rc_: e.tensor_scalar(out=rc_, in0=sc_, scalar1=1.0 / 2048, scalar2=EPS,
                                                               op0=ALU.mult, op1=ALU.add),
             r=[('I', i, 'ss')], w=[('I', i, 'rs')])
        S.op('act', lambda e, rc_=rc_: e.activation(out=rc_, in_=rc_, func=AF.Sqrt), r=[('I', i, 'rs')],
             w=[('I', i, 'rs')])
        S.op('dve', lambda e, rc_=rc_: e.reciprocal(out=rc_, in_=rc_), r=[('I', i, 'rs')], w=[('I', i, 'rs')])
        S.op('dve', lambda e, p2=p2, rc_=rc_: e.scalar_tensor_tensor(
            out=yo[p2][:], in0=yo[p2][:], scalar=rc_, in1=w3b[:], op0=ALU.mult, op1=ALU.mult),
            r=[('yo', p2), ('I', i, 'rs'), 'w3b'], w=[('yo', p2)])
        outs.append(S.op('sp', lambda e, i=i, p2=p2: e.dma_start(out=y[i * 128:(i + 1) * 128, :], in_=yo[p2][:]),
                         r=[('yo', p2)], w=[('y', i)], kind='d'))
    return finish()


_PROG = {}


def _col(v):
    return np.ascontiguousarray(np.asarray(v, np.float32).reshape(-1, 128).T)


def _host_tables(na_rpb):
    rpb = np.asarray(na_rpb, np.float32)
    a = np.arange(2)[:, None, None, None, None]
    kc = np.arange(64)[None, :, None, None, None]
    bq = np.arange(2)[None, None, None, :, None]
    cq = np.arange(64)[None, None, None, None, :]
    cs = np.clip(cq - 8, 0, 48)
    colok = (kc >= cs) & (kc <= cs + 15)
    coff = np.clip(kc - cq + 15, 0, 30)
    tabs = []
    for s in range(4):
        r0 = 16 * s
        T = np.full((8, 8, 2, 64, 6, 2, 64), NEG, np.float32)
        for m in range(8):
            nl = nl_list(m)
            n = np.array(nl)[None, None, :, None, None]
            u = 2 * n + a
            rho = 2 * m + bq
            kr = r0 - 4 + u
            r = r0 + rho
            rsr = np.clip(r - 4, 0, 56)
            rowok = (kr >= rsr) & (kr <= rsr + 7) & (kr >= 0) & (kr <= 63)
            roff = np.clip(kr - r + 7, 0, 14)
            ok = np.broadcast_to(rowok & colok, (2, 64, 6, 2, 64))
            ro = np.broadcast_to(roff, (2, 64, 6, 2, 64))
            co = np.broadcast_to(coff, (2, 64, 6, 2, 64))
            for h in range(8):
                T[h, m] = np.where(ok, rpb[h][ro, co], np.float32(NEG))
        tabs.append(T.reshape(8, 8, 128, 6, 128))
    return tabs


def _const_tables():
    p = np.arange(128, dtype=np.float64)
    inv = 10000.0 ** (-(np.arange(64, dtype=np.float64)) / 64.0)
    ropes = []
    for s in range(4):
        t = np.arange(1024)
        row = 16 * s + t // 64
        col = t % 64
        ang = np.zeros((128, 1024))
        ang[0:64] = inv[:, None] * row[None, :]
        ang[64:128] = inv[:, None] * col[None, :]
        ropes.append(np.stack([np.cos(ang), np.sin(ang)], axis=1).astype(np.float32))
    rtabs = []
    for s in range(4):
        R = np.zeros((128, 30), np.float64)
        R[:, 0] = 127 - p
        R[:, 1] = p + 1
        R[:, 2] = 128
        R[:, 3] = 255 - p
        R[:, 4] = 127 - p
        R[:, 10] = p
        R[:, 11] = 128 - p
        R[:, 12] = 128
        R[:, 13] = p
        R[:, 14] = 128 + p
        R[:, 5] = 1024 * s
        R[:, 20] = 1.0
        R[:, 15] = 1024 * (3 - s)
        R[:, 25] = 1.0
        for i in range(4):
            if i < s:
                R[:, 6 + i] = 1024 * (s - 1 - i)
                R[:, 21 + i] = 1.0
            if i > s:
                R[:, 16 + i] = 1024 * (i - s - 1)
                R[:, 26 + i] = 1.0
        rtabs.append(R.astype(np.float32))
    j = np.arange(128)[:, None]
    i = np.arange(128)[None, :]
    dm = np.zeros((128, 4, 128), np.float32)
    dm[:, 0, :] = np.maximum(i - j, 0)
    dm[:, 1, :] = np.maximum(j - i, 0)
    dm[:, 2, :] = (i >= j) / 16.0
    dm[:, 3, :] = (j >= i) / 16.0
    return ropes, rtabs, dm


def kernel(x, c, ctx, c_ctx, w_mod, b_mod, norm_mix_w, w_in, ret_decay_f, ret_decay_b, ret_gn_w, na_rpb, w_out,
           norm_ffn_w, w_router_group, b_router_group, w_router_expert, b_router_expert, w_gate, w_up, w_down,
           final_norm_w):
    if 'nc' not in _PROG:
        _PROG['nc'] = build_program()
    nc = _PROG['nc']
    in_maps = make_inputs(x, c, ctx, c_ctx, w_mod, b_mod, norm_mix_w, w_in, ret_decay_f, ret_decay_b, ret_gn_w, na_rpb,
                          w_out, norm_ffn_w, w_router_group, b_router_group, w_router_expert, b_router_expert, w_gate,
                          w_up, w_down, final_norm_w)
    res = run_bass_kernel_spmd(nc, in_maps, core_ids=list(range(8)))
    out = np.zeros((2, 4096, 2048), np.float32)
    for core in range(8):
        b, s = core // 4, core % 4
        out[b, s * 1024:(s + 1) * 1024] = res.results[core]["y"]
    return out


def make_inputs(x, c, ctx, c_ctx, w_mod, b_mod, norm_mix_w, w_in, ret_decay_f, ret_decay_b, ret_gn_w, na_rpb, w_out,
                norm_ffn_w, w_router_group, b_router_group, w_router_expert, b_router_expert, w_gate, w_up, w_down,
                final_norm_w):
    f = lambda a: np.asarray(a, np.float32)
    x, c, ctx, c_ctx = f(x), f(c), f(ctx), f(c_ctx)
    perm = np.arange(7168)
    for base in list(range(0, 1024, 256)) + list(range(1024, 2048, 256)):
        perm[base:base + 256] = base + np.concatenate([np.arange(0, 64), np.arange(128, 192), np.arange(64, 128),
                                                       np.arange(192, 256)])
    win = np.ascontiguousarray(f(w_in)[0][:, perm])
    wmod = f(w_mod)[0]
    wout = f(w_out)[0]
    wrt = np.ascontiguousarray(np.concatenate(
        [f(w_router_group)[0], np.transpose(f(w_router_expert)[0], (1, 0, 2)).reshape(2048, 32)], axis=1))
    brt = np.concatenate([f(b_router_group)[0].reshape(-1), f(b_router_expert)[0].reshape(-1)])
    wg, wu, wd = f(w_gate)[0], f(w_up)[0], f(w_down)[0]
    fnw = f(final_norm_w)
    gnw = f(ret_gn_w)[0]
    dec = np.concatenate([f(ret_decay_f)[0], f(ret_decay_b)[0]])
    tcs = _host_tables(f(na_rpb)[0])
    ropes, rtabs, dm = _const_tables()
    bmodc = _col(f(b_mod)[0])
    nw1c = _col(f(norm_mix_w)[0])
    nw2c = _col(f(norm_ffn_w)[0])
    in_maps = []
    for core in range(8):
        b, s = core // 4, core % 4
        slab = np.zeros((24, 64, 2048), np.float32)
        xb = x[b].reshape(64, 64, 2048)
        lo, hi = 16 * s - 4, 16 * s + 20
        slo, shi = max(lo, 0), min(hi, 64)
        slab[slo - lo:shi - lo] = xb[slo:shi]
        ccol = np.stack([_col(c[b]), _col(c_ctx)], axis=2).reshape(128, 32)
        colp = np.ascontiguousarray(np.concatenate([ccol, bmodc, nw1c, nw2c], axis=1))
        in_maps.append({
            "xs": slab.reshape(1536, 2048), "ctxb": np.ascontiguousarray(ctx[b]), "colp": colp, "wmod": wmod,
            "win": win, "wout": wout, "wrt": wrt, "brt": brt, "wg": wg, "wu": wu, "wd": wd, "fnw": fnw, "gnw": gnw,
            "dec": dec, "ropet": ropes[s], "tcd": tcs[s], "rtab": rtabs[s], "dmk": dm,
        })
    return in_maps
```

```python
import numpy as np
import concourse.bass as bass
import concourse.mybir as mybir
from concourse.bass_utils import run_bass_kernel_spmd

F32 = mybir.dt.float32
BF16 = mybir.dt.bfloat16
AF = mybir.ActivationFunctionType
ALU = mybir.AluOpType
AX = mybir.AxisListType

NEG = -30000.0
EPS = 1e-6
SB_BASE = 16640
SB_END = 16512 + 212863

DEBUG = {}


class Sch:
    def __init__(self, nc, n_dma_sems=40):
        self.nc = nc
        self.E = {'pe': nc.tensor, 'act': nc.scalar, 'dve': nc.vector, 'pool': nc.gpsimd, 'sp': nc.sync}
        self.ops = []
        self.lastw = {}
        self.readers = {}
        self.bar_deps = {}
        self.open_async = set()
        self.last_on = {}
        self.n_dma_sems = n_dma_sems

    def op(self, eng, fn, r=(), w=(), kind='c'):
        i = len(self.ops)
        deps = {}
        psr = [k_ for k_ in r if isinstance(k_, tuple) and len(k_) == 2 and k_[0] == 'ps']
        if psr:
            r = [k_ for k_ in r if k_ not in psr]
            w = list(w) + [k_ for k_ in psr if k_ not in w]

        def add(d, k):
            if d is None or d == i:
                return
            if d in deps and (deps[d] != 'war' or k == 'war'):
                return
            deps[d] = k

        for b in r:
            add(self.lastw.get(b), 'raw')
        for b in w:
            add(self.lastw.get(b), 'waw')
            rd = self.readers.get(b)
            if rd:
                for d in rd[0].values():
                    add(d, 'war')
                for d in rd[1]:
                    add(d, 'war')
        bd = self.bar_deps.pop(eng, None)
        if bd:
            for d in bd:
                add(d, 'raw')
        for b in w:
            self.lastw[b] = i
            self.readers[b] = ({}, [])
        for b in r:
            rd = self.readers.setdefault(b, ({}, []))
            if kind == 'c':
                rd[0][eng] = i
            else:
                rd[1].append(i)
        for d in deps:
            self.open_async.discard(d)
        if kind != 'c':
            self.open_async.add(i)
        self.ops.append((eng, fn, deps, kind))
        self.last_on[eng] = i
        return i

    def bar(self):
        deps = set(self.last_on.values()) | set(self.open_async)
        self.open_async.clear()
        for e in self.E:
            self.bar_deps[e] = set(deps) | self.bar_deps.get(e, set())

    def emit(self, stack):
        nc = self.nc
        ops = self.ops
        n = len(ops)
        need = [False] * n
        for i, (eng, fn, deps, kind) in enumerate(ops):
            for d, k in deps.items():
                pe_, _, _, pk = ops[d]
                if pk != 'c':
                    continue
                if pe_ == eng and eng == 'pe':
                    continue
                need[d] = True
        psem = {e: stack.enter_context(nc.semaphore("pg_" + e)) for e in self.E}
        half_n = self.n_dma_sems // 2
        dsem = [stack.enter_context(nc.semaphore("dm_%d" % j)) for j in range(self.n_dma_sems)]
        duse = [0] * self.n_dma_sems
        ndma_q = {'pool': 0, 'other': 0}
        pcnt = {e: 0 for e in self.E}
        sig = [None] * n
        waited = {e: {} for e in self.E}
        ndma = 0
        for i, (eng, fn, deps, kind) in enumerate(ops):
            E = self.E[eng]
            wl = {}
            for d, k in deps.items():
                pe_, _, _, pk = ops[d]
                if pk == 'c' and pe_ == eng and eng == 'pe':
                    continue
                s, v = sig[d]
                key = id(s)
                if waited[eng].get(key, 0) >= v:
                    continue
                if key not in wl or wl[key][1] < v:
                    wl[key] = (s, v)
            my = None
            if kind == 'd':
                qk = 'pool' if eng == 'pool' else 'other'
                j = (ndma_q[qk] % half_n) + (0 if qk == 'pool' else half_n)
                ndma_q[qk] += 1
                ndma += 1
                s = dsem[j]
                if duse[j] > 0:
                    key = id(s)
                    pv = 16 * duse[j]
                    if waited[eng].get(key, 0) < pv and (key not in wl or wl[key][1] < pv):
                        wl[key] = (s, pv)
                duse[j] += 1
                my = (s, 16 * duse[j], 16)
            elif kind == 'cc':
                s = stack.enter_context(nc.semaphore("cc_%d" % i))
                my = (s, 1, 1)
            elif need[i]:
                pcnt[eng] += 1
                my = (psem[eng], pcnt[eng], 1)
            for key, (s, v) in wl.items():
                E.wait_ge(s, v)
                waited[eng][key] = v
            ins = fn(E)
            if my is not None:
                ins.then_inc(my[0], my[2])
                sig[i] = (my[0], my[1])
        return


class Arena:
    def __init__(self, nc):
        self.nc = nc
        self.off = SB_BASE
        self.n = 0

    def alloc(self, shape, dtype, name=None):
        sz = 1
        for s in shape[1:]:
            sz *= s
        sz *= 2 if dtype == BF16 else 4
        off = (self.off + 63) // 64 * 64
        assert off + sz <= min(SB_END, getattr(self, 'limit', SB_END)), ("SBUF overflow", name, off, sz, self.limit)
        self.n += 1
        t = self.nc.alloc_sbuf_tensor_at("%s_%d" % (name or "t", self.n), list(shape), dtype, offset=off)
        self.off = off + sz
        return t

    def seek(self, kb, limit_kb):
        self.off = SB_BASE + int(kb * 1024)
        self.limit = SB_BASE + int(limit_kb * 1024)


def nl_list(m):
    return list(range(m, m + 6)) if m <= 6 else list(range(6, 12))


def build_program(stop=None, dbg=()):
    nc = bass.Bass("TRN2", target_bir_lowering=False)
    S = Sch(nc)
    A = Arena(nc)

    _shapes = {"xs": [1536, 2048], "ctxb": [256, 2048], "colp": [128, 160], "wmod": [2048, 12288],
               "win": [2048, 7168], "wout": [2048, 2048], "wrt": [2048, 36], "brt": [36], "wg": [32, 2048, 512],
               "wu": [32, 2048, 512], "wd": [32, 512, 2048], "fnw": [2048], "gnw": [1024], "dec": [8],
               "ropet": [128, 2, 1024], "tcd": [8, 8, 128, 6, 128], "rtab": [128, 30], "dmk": [128, 4, 128]}
    _decl = {}

    class _Lazy:
        def __init__(self, name):
            self.name = name

        def ap(self):
            if self.name not in _decl:
                _decl[self.name] = nc.dram_tensor(self.name, list(_shapes[self.name]), F32, kind="ExternalInput").ap()
            return _decl[self.name]

        def __getitem__(self, key):
            return self.ap()[key]

        def rearrange(self, *a, **k):
            return self.ap().rearrange(*a, **k)

        def partition_broadcast(self, n):
            return self.ap().partition_broadcast(n)

    xs, ctxb, colp_d, wmod, win, wout, wrt, brt, wg, wu, wd, fnw, gnw, dec, ropet_d, tcd, rtab_d, dmk_d = [
        _Lazy(n) for n in ["xs", "ctxb", "colp", "wmod", "win", "wout", "wrt", "brt", "wg", "wu", "wd", "fnw", "gnw",
                           "dec", "ropet", "tcd", "rtab", "dmk"]]
    nc._declared_inputs = _decl
    y = nc.dram_tensor("y", [1024, 2048], F32, kind="ExternalOutput").ap()
    x1s = nc.dram_tensor("x1s", [1024, 2048], F32).ap()
    stin = [nc.dram_tensor("stin%d" % h, [512, 256], F32) for h in range(4)]
    stall = [nc.dram_tensor("stall%d" % h, [2048, 256], F32) for h in range(4)]
    dbg_n = [0]

    def dump(name, ap, shape, keys, dtype=F32):
        if name not in dbg:
            return
        d = nc.dram_tensor("dbg_" + name, list(shape), dtype, kind="ExternalOutput").ap()
        S.op('sp', lambda e: e.dma_start(out=d, in_=ap), r=keys, w=[('dbg', name)], kind='d')

    def finish():
        S.bar()
        S.op('sp', lambda e: e.nop(), r=[], w=[])
        from contextlib import ExitStack
        with ExitStack() as es:
            S.emit(es)
        return nc

    PS = nc.alloc_psum_tensor("PS", [128, 8, 512], F32)

    def psb(b):
        return PS[:, b, :]

    def pskey(b):
        return ('ps', b)

    A.seek(0, 6)
    ident = A.alloc([128, 128], BF16, "ident")
    identf = A.alloc([128, 128], F32, "identf")
    colp = A.alloc([128, 160], F32, "colp")
    modc = A.alloc([128, 96, 2], F32, "modc")
    ABc = A.alloc([128, 3, 16], F32, "ABc")
    sc = A.alloc([128, 16, 2], BF16, "sc")
    small = A.alloc([128, 64], F32, "small")
    rsm = A.alloc([128, 64], F32, "rsm")
    comb = A.alloc([128, 8, 32], F32, "comb")
    wrb = A.alloc([128, 16, 36], BF16, "wrb")
    brb = A.alloc([128, 36], F32, "brb")
    lgt = A.alloc([128, 36], F32, "lgt")
    A.seek(6, 38)
    cat = A.alloc([128, 8, 2048], BF16, "cat")

    S.op('pool', lambda e: e.memset(ident[:], 1.0), w=['ident'])
    S.op('pool', lambda e: e.affine_select(out=ident[:], in_=ident[:], pattern=[[-1, 128]], compare_op=ALU.is_equal,
                                           fill=0.0, base=0, channel_multiplier=1), r=['ident'], w=['ident'])
    S.op('pool', lambda e: e.memset(identf[:], 1.0), w=['identf'])
    S.op('pool', lambda e: e.affine_select(out=identf[:], in_=identf[:], pattern=[[-1, 128]], compare_op=ALU.is_equal,
                                           fill=0.0, base=0, channel_multiplier=1), r=['identf'], w=['identf'])
    S.op('sp', lambda e: e.dma_start(out=colp[:], in_=colp_d.ap()), w=['colp'], kind='d')
    ccol = colp[:, 0:32]
    bmodc = colp[:, 32:128]
    nw1c = colp[:, 128:144]
    nw2c = colp[:, 144:160]
    S.op('act', lambda e: e.activation(out=sc[:].rearrange("p k c -> p (k c)"), in_=ccol, func=AF.Silu),
         r=['colp'], w=['sc'])

    A.seek(38, 126)
    hTa = A.alloc([128, 16, 1280], BF16, "hTa")
    ring = [A.alloc([128, 16, 256], BF16, "ring%d" % i) for i in range(4)]
    hTb = A.alloc([128, 16, 512], BF16, "hTb")
    A.seek(126, 175.5)

    def hloc(t):
        if 2 <= t <= 9:
            return hTa, (t - 2) * 128
        if t >= 12:
            return hTa, 1024 + (t - 12) * 128
        if t < 2:
            return hTb, t * 128
        return hTb, (t - 8) * 128

    def hTk(t, k):
        H, c0 = hloc(t)
        return H[:, k, c0:c0 + 128]

    if stop == '0':
        dump('sc', sc[:], [128, 16, 2], ['sc'], BF16)
        return finish()
    A.seek(175.5, 207.5)
    mring = [A.alloc([128, 16, 256], BF16, "mring%d" % i) for i in range(4)]
    A.seek(126, 175.5)

    def modA_dma(t):
        sl, key = (ring[t % 4], 'ring') if t < 16 else (mring[t % 4], 'mring')
        S.op('pool', lambda e, sl=sl, t=t: e.dma_start(
            out=sl[:], in_=wmod[:, t * 256:(t + 1) * 256].rearrange("(k p) n -> p k n", p=128)),
            w=[(key, t % 4, 0), (key, t % 4, 1)], kind='d')

    def modA_mm(t):
        sl, key = (ring[t % 4], 'ring') if t < 16 else (mring[t % 4], 'mring')
        for jj in range(2):
            j = 2 * t + jj
            bank = 0 if j < 32 else 6
            jl = j if j < 32 else j - 32 + 128
            for k in range(16):
                S.op('pe', lambda e, sl=sl, jj=jj, k=k, bank=bank, jl=jl: e.matmul(
                    psb(bank)[:, jl * 2:jl * 2 + 2], lhsT=sl[:, k, jj * 128:(jj + 1) * 128], rhs=sc[:, k, :],
                    start=(k == 0), stop=(k == 15)),
                    r=[(key, t % 4, jj), 'sc'], w=[pskey(bank)])

    def modA_tail_finish():
        S.op('dve', lambda e: e.tensor_tensor(
            out=modc[:, 32:96, 0], in0=psb(6)[:, 256:384].rearrange("p (j c) -> p j c", c=2)[:, :, 0],
            in1=bmodc[:, 32:96], op=ALU.add), r=[pskey(6), 'colp'], w=[('modc', 1, 0)])
        S.op('dve', lambda e: e.scalar_tensor_tensor(out=ABc[:, 2, :], in0=modc[:, 64:80, 0], scalar=1.0, in1=nw2c,
                                                     op0=ALU.add, op1=ALU.mult),
             r=[('modc', 1, 0), 'colp'], w=['A2'])

    nA = 16
    for t in range(nA):
        modA_dma(t)
        modA_mm(t)
        if t == 15:
            for c in range(2):
                S.op('dve', lambda e, c=c: e.tensor_tensor(
                    out=modc[:, 0:32, c], in0=psb(0)[:, 0:64].rearrange("p (j c) -> p j c", c=2)[:, :, c],
                    in1=bmodc[:, 0:32], op=ALU.add), r=[pskey(0), 'colp'], w=[('modc', 0, c)])
            S.op('dve', lambda e: e.scalar_tensor_tensor(out=ABc[:, 0, :], in0=modc[:, 16:32, 0], scalar=1.0, in1=nw1c,
                                                         op0=ALU.add, op1=ALU.mult),
                 r=[('modc', 0, 0), 'colp'], w=['A1'])
            S.op('dve', lambda e: e.scalar_tensor_tensor(out=ABc[:, 1, :], in0=modc[:, 16:32, 1], scalar=1.0, in1=nw1c,
                                                         op0=ALU.add, op1=ALU.mult),
                 r=[('modc', 0, 1), 'colp'], w=['Ac'])
    dump('modc', modc[:], [128, 96, 2], [('modc', 0, 0), ('modc', 0, 1), ('modc', 1, 0)])
    if stop == 'A':
        for t in range(16, 48):
            modA_dma(t)
            modA_mm(t)
        modA_tail_finish()
        return finish()
    xin = [A.alloc([128, 2048], F32, "xin%d" % i) for i in range(2)]
    xn = [A.alloc([128, 2048], BF16, "xn%d" % i) for i in range(2)]
    ss = A.alloc([128, 16], F32, "ss")
    rs = A.alloc([128, 16], F32, "rs")
    S.op('pool', lambda e: e.memset(ss[:], 0.0), w=['ss'])

    def rms_tile(junk_t, junk_key, xin_t, xin_key, ss_col, rs_col, tag):
        S.op('act', lambda e: e.activation(out=junk_t[:], in_=xin_t[:], func=AF.Square, accum_out=ss_col),
             r=[xin_key, 'ss'], w=[junk_key, (tag, 'ss')])
        S.op('dve', lambda e: e.tensor_scalar(out=rs_col, in0=ss_col, scalar1=1.0 / 2048, scalar2=EPS, op0=ALU.mult,
                                              op1=ALU.add), r=[(tag, 'ss')], w=[(tag, 'rs')])
        S.op('act', lambda e: e.activation(out=rs_col, in_=rs_col, func=AF.Sqrt), r=[(tag, 'rs')], w=[(tag, 'rs')])
        S.op('dve', lambda e: e.reciprocal(out=rs_col, in_=rs_col), r=[(tag, 'rs')], w=[(tag, 'rs')])

    def transpose_mod(src_bf, src_key, dst_fn, dst_keys, Acol, Bcol, Akey, Bkey, pbank):
        pst = PS[:, pbank:pbank + 2, :].rearrange("p b f -> p (b f)").bitcast(BF16)
        pst = pst.rearrange("p (k t) -> p k t", t=128)
        for k in range(16):
            S.op('pe', lambda e, k=k: e.transpose(pst[:, k, :], src_bf[:, k * 128:(k + 1) * 128], ident[:]),
                 r=[src_key, 'ident'], w=[pskey(pbank + k // 8)])
        for k in range(16):
            if k < 8:
                S.op('act', lambda e, k=k: e.activation(out=dst_fn(k), in_=pst[:, k, :], func=AF.Identity,
                                                        scale=Acol(k), bias=Bcol(k)),
                     r=[pskey(pbank + k // 8), Akey, Bkey], w=[dst_keys(k)])
            else:
                S.op('dve', lambda e, k=k: e.tensor_scalar(out=dst_fn(k), in0=pst[:, k, :], scalar1=Acol(k),
                                                           scalar2=Bcol(k), op0=ALU.mult, op1=ALU.add),
                     r=[pskey(pbank + k // 8), Akey, Bkey], w=[dst_keys(k)])

    for t in range(14):
        xi = xin[t % 2]
        xk = ('xin', t % 2)
        src = xs[t * 128:(t + 1) * 128, :] if t < 12 else ctxb[(t - 12) * 128:(t - 11) * 128, :]
        S.op('sp', lambda e, xi=xi, src=src: e.dma_start(out=xi[:], in_=src), w=[xk], kind='d')
        xnt = xn[t % 2]
        rms_tile(xnt, ('xn', t % 2), xi, xk, ss[:, t:t + 1], rs[:, t:t + 1], ('B', t))
        S.op('dve', lambda e, xi=xi, xnt=xnt, t=t: e.tensor_scalar(out=xnt[:], in0=xi[:], scalar1=rs[:, t:t + 1],
                                                                   scalar2=None, op0=ALU.mult),
             r=[xk, (('B', t), 'rs')], w=[('xn', t % 2)])
        ai = 0 if t < 12 else 1
        ci = 0 if t < 12 else 1
        transpose_mod(xnt, ('xn', t % 2),
                      lambda k, t=t: hTk(t, k),
                      lambda k, t=t: ('hT', t),
                      lambda k, ai=ai: ABc[:, ai, k:k + 1],
                      lambda k, ci=ci: modc[:, k, ci:ci + 1],
                      'A1' if t < 12 else 'Ac', ('modc', 0, ci), 2 + 2 * (t % 2))

    hT_all = [('hT', t) for t in range(14)]
    dump('hTa', hTa[:], [128, 16, 1280], hT_all, BF16)
    dump('hTb', hTb[:], [128, 16, 512], hT_all, BF16)
    if stop == 'B':
        return finish()
    NA_REGION_END = 175.5
    hT_own = [('hT', t) for t in range(2, 10)]
    A.seek(126, 175.5)
    S.bar()

    qTn = [A.alloc([128, 1024], BF16, "qTn%d" % i) for i in range(2)]
    kTn = [A.alloc([128, 1792], BF16, "kTn%d" % i) for i in range(2)]
    vA = [A.alloc([128, 14, 130], BF16, "vA%d" % i) for i in range(2)]
    tcb = [A.alloc([128, 6, 128], F32, "tcb%d" % i) for i in range(2)]
    tmpS = [A.alloc([128, 6, 128], F32, "tmpS%d" % i) for i in range(2)]
    PT = [A.alloc([128, 8, 128], BF16, "PT%d" % i) for i in range(2)]
    for i in range(2):
        S.op('pool', lambda e, i=i: e.memset(vA[i][:, :, 128:130], 1.0), w=[('vA1', i)])

    rc = [0]
    SC_NA = float(128 ** -0.5)
    def wload(slot, half, col):
        S.op('pool', lambda e: e.dma_start(out=ring[slot][:, :, half * 128:(half + 1) * 128],
                                           in_=win[:, col:col + 128].rearrange("(k p) n -> p k n", p=128)),
             w=[('ring', slot, half)], kind='d')

    def wload_head(hh):
        t0, t1 = (2 * hh) % 4, (2 * hh + 1) % 4
        wload(t0, 0, 4096 + hh * 128)
        wload(t0, 1, 5120 + hh * 128)
        wload(t1, 0, 6144 + hh * 128)

    kgroups = [(hTb, 0, 512, [(0, 0, 256), (1280, 256, 256)]), (hTa, 0, 512, [(256, 0, 512)]),
               (hTa, 512, 512, [(768, 0, 512)]), (hTa, 1024, 256, [(1536, 0, 256)])]

    def inproj_groups(h):
        b = h % 2
        s0 = (2 * h) % 4
        s1 = (2 * h + 1) % 4
        r0k = ('ring', s0, 0)
        r1k = ('ring', s0, 1)
        r2k = ('ring', s1, 0)
        wq = ring[s0][:, :, 0:128]
        wk = ring[s0][:, :, 128:256]
        wv = ring[s1][:, :, 0:128]
        gl = []

        def g_q(half):
            bank = rc[0] % 2
            rc[0] += 1
            for k in range(16):
                S.op('pe', lambda e, k=k, half=half, bank=bank: e.matmul(
                    psb(bank), lhsT=wq[:, k, :], rhs=hTa[:, k, half * 512:(half + 1) * 512],
                    start=(k == 0), stop=(k == 15)), r=[r0k] + hT_own, w=[pskey(bank)])
            S.op('act', lambda e, half=half, bank=bank: e.activation(
                out=qTn[b][:, half * 512:(half + 1) * 512], in_=psb(bank), func=AF.Copy, scale=SC_NA),
                r=[pskey(bank)], w=[('qTn', b)])

        def g_k(H_, c0, n, dsts):
            bank = rc[0] % 2
            rc[0] += 1
            for k in range(16):
                S.op('pe', lambda e, k=k, bank=bank: e.matmul(
                    psb(bank)[:, 0:n], lhsT=wk[:, k, :], rhs=H_[:, k, c0:c0 + n],
                    start=(k == 0), stop=(k == 15)), r=[r1k] + hT_all, w=[pskey(bank)])
            for (d0, s0_, nn) in dsts:
                S.op('dve', lambda e, bank=bank, d0=d0, s0_=s0_, nn=nn: e.tensor_copy(
                    out=kTn[b][:, d0:d0 + nn], in_=psb(bank)[:, s0_:s0_ + nn]), r=[pskey(bank)], w=[('kTn', b)])

        def g_v(g):
            bank = rc[0] % 2
            rc[0] += 1
            nt = 4 if g < 3 else 2
            for tt in range(nt):
                t = g * 4 + tt
                for k in range(16):
                    S.op('pe', lambda e, k=k, t=t, tt=tt, bank=bank: e.matmul(
                        psb(bank)[:, tt * 128:(tt + 1) * 128], lhsT=hTk(t, k), rhs=wv[:, k, :],
                        start=(k == 0), stop=(k == 15)), r=[r2k, ('hT', t)], w=[pskey(bank)])
            S.op('act', lambda e, g=g, bank=bank, nt=nt: e.activation(
                out=vA[b][:, g * 4:g * 4 + nt, 0:128],
                in_=psb(bank)[:, 0:nt * 128].rearrange("p (t c) -> p t c", c=128), func=AF.Copy),
                r=[pskey(bank)], w=[('vA', b)])

        for half in range(2):
            gl.append(lambda half=half: g_q(half))
        for kg in kgroups:
            gl.append(lambda kg=kg: g_k(*kg))
        for g in range(4):
            gl.append(lambda g=g: g_v(g))
        return gl

    def attn_S(h, m):
        b = h % 2
        p2 = (h * 8 + m) % 2
        sb0 = 2 + 2 * p2
        psS = PS[:, sb0:sb0 + 2, :].rearrange("p b (t c) -> p (b t) c", c=128)
        tiles = nl_list(m) + [12, 13]
        S.op('sp', lambda e: e.dma_start(out=tcb[p2][:], in_=tcd[h, m]), w=[('tcb', p2)], kind='d')
        for w_, n in enumerate(tiles):
            S.op('pe', lambda e, w_=w_, n=n: e.matmul(
                psS[:, w_, :], lhsT=kTn[b][:, n * 128:(n + 1) * 128], rhs=qTn[b][:, m * 128:(m + 1) * 128],
                start=True, stop=True), r=[('kTn', b), ('qTn', b)], w=[pskey(sb0 + w_ // 4)])
        S.op('dve', lambda e: e.tensor_tensor(out=tmpS[p2][:, 0:4, :], in0=psS[:, 0:4, :],
                                              in1=tcb[p2][:, 0:4, :], op=ALU.add),
             r=[pskey(sb0), ('tcb', p2)], w=[('tmpS', p2, 0)])
        S.op('dve', lambda e: e.tensor_tensor(out=tmpS[p2][:, 4:6, :], in0=psS[:, 4:6, :],
                                              in1=tcb[p2][:, 4:6, :], op=ALU.add),
             r=[pskey(sb0 + 1), ('tcb', p2)], w=[('tmpS', p2, 1)])
        S.op('act', lambda e: e.activation(out=PT[p2][:, 0:6, :], in_=tmpS[p2][:], func=AF.Exp),
             r=[('tmpS', p2, 0), ('tmpS', p2, 1)], w=[('PT', p2, 0)])
        S.op('act', lambda e: e.activation(out=PT[p2][:, 6:8, :], in_=psS[:, 6:8, :], func=AF.Exp),
             r=[pskey(sb0 + 1)], w=[('PT', p2, 1)])

    def attn_PV(h, m):
        b = h % 2
        p2 = (h * 8 + m) % 2
        pO = psb(6 + p2)
        tiles = nl_list(m) + [12, 13]
        for w_, n in enumerate(tiles):
            S.op('pe', lambda e, w_=w_, n=n: e.matmul(
                pO[:, 0:130], lhsT=PT[p2][:, w_, :], rhs=vA[b][:, n, :], start=(w_ == 0), stop=(w_ == 7)),
                r=[('PT', p2, 0), ('PT', p2, 1), ('vA', b), ('vA1', b)], w=[pskey(6 + p2)])
        rcol = small[:, p2:p2 + 1]
        S.op('dve', lambda e: e.reciprocal(out=rcol, in_=pO[:, 128:129]), r=[pskey(6 + p2)], w=[('rcol', p2)])
        S.op('act', lambda e: e.activation(
            out=cat[:, m, 1024 + h * 128:1024 + (h + 1) * 128], in_=pO[:, 0:128], func=AF.Copy, scale=rcol),
            r=[pskey(6 + p2), ('rcol', p2)], w=[('cat', m)])

    wload_head(0)
    wload_head(1)
    for g_ in inproj_groups(0):
        g_()
    for h in range(8):
        if h + 2 < 8:
            wload_head(h + 2)
        for tt_ in range(4):
            modA_dma(16 + 4 * h + tt_)
        nxt = inproj_groups(h + 1) if h + 1 < 8 else []
        for m in range(8):
            attn_S(h, m)
            for _ in range(2 if m < 2 else 1):
                if nxt:
                    nxt.pop(0)()
            attn_PV(h, m)
        while nxt:
            nxt.pop(0)()
        for tt_ in range(4):
            modA_mm(16 + 4 * h + tt_)

    modA_tail_finish()

    dump('cat', cat[:], [128, 8, 2048], [('cat', m) for m in range(8)], BF16)
    dump('hTb2', hTb[:], [128, 16, 512], hT_all, BF16)
    dump('kTn1', kTn[1][:], [128, 1792], [('kTn', 1)], BF16)
    dump('vA1', vA[1][:], [128, 14, 130], [('vA', 1), ('vA1', 1)], BF16)
    dump('qTn1', qTn[1][:], [128, 1024], [('qTn', 1)], BF16)
    if stop == 'D':
        return finish()
    A.seek(110, 207.5)
    S.bar()

    ropet = A.alloc([128, 2, 1024], F32, "ropet")
    rtab = A.alloc([128, 30], F32, "rtab")
    dmk = A.alloc([128, 4, 128], F32, "dmk")
    decb = A.alloc([128, 8], F32, "decb")
    lg = A.alloc([128, 8], F32, "lg")
    dc = A.alloc([128, 4, 20], F32, "dc")
    gnb = A.alloc([128, 1024], F32, "gnb")
    Dfb = A.alloc([128, 128], F32, "Dfb")
    e12 = A.alloc([128, 2, 128], F32, "e12")
    qT = A.alloc([128, 2, 1024], BF16, "qT")
    kT = A.alloc([128, 2, 1024], BF16, "kT")
    ktm = [A.alloc([128, 8, 256], BF16, "ktm%d" % d) for d in range(2)]
    vtm = A.alloc([128, 8, 256], BF16, "vtm")
    gs = A.alloc([128, 8, 256], BF16, "gs")
    cktm = [A.alloc([128, 2, 256], BF16, "cktm%d" % d) for d in range(2)]
    cvtm = A.alloc([128, 2, 256], BF16, "cvtm")
    St = [A.alloc([128, 2, 256], F32, "St%d" % d) for d in range(2)]
    Sbf = [A.alloc([128, 8, 2, 256], BF16, "Sbf%d" % d) for d in range(2)]
    G = [A.alloc([128, 4, 256], F32, "G%d" % i) for i in range(2)]
    rt = [A.alloc([128, 512], F32, "rt%d" % i) for i in range(4)]
    PTr = [A.alloc([128, 128], BF16, "PTr%d" % i) for i in range(2)]
    oall = A.alloc([128, 8, 256], F32, "oall")
    bst = A.alloc([128, 8, 6], F32, "bst")
    mv = A.alloc([128, 8, 2], F32, "mv")
    rstd_r = A.alloc([128, 8], F32, "rstd_r")
    ynorm = [A.alloc([128, 256], F32, "ynorm%d" % i) for i in range(2)]

    S.op('sp', lambda e: e.dma_start(out=ropet[:], in_=ropet_d.ap()), w=['ropet'], kind='d')
    S.op('sp', lambda e: e.dma_start(out=rtab[:], in_=rtab_d.ap()), w=['rtab'], kind='d')
    S.op('sp', lambda e: e.dma_start(out=dmk[:], in_=dmk_d.ap()), w=['dmk'], kind='d')
    S.op('sp', lambda e: e.dma_start(out=decb[:], in_=dec.partition_broadcast(128)), w=['decb'], kind='d')
    S.op('sp', lambda e: e.dma_start(out=gnb[:], in_=gnw.partition_broadcast(128)), w=['gnb'], kind='d')
    S.op('act', lambda e: e.activation(out=lg[:], in_=decb[:], func=AF.Exp, scale=-1.0), r=['decb'], w=['lg'])
    S.op('act', lambda e: e.activation(out=lg[:], in_=lg[:], func=AF.Ln, bias=1.0), r=['lg'], w=['lg'])
    S.op('dve', lambda e: e.tensor_scalar(out=lg[:], in0=lg[:], scalar1=-1.0, scalar2=None, op0=ALU.mult),
         r=['lg'], w=['lg'])
    for h in range(4):
        S.op('act', lambda e, h=h: e.activation(out=dc[:, h, 0:10], in_=rtab[:, 0:10], func=AF.Exp,
                                                scale=lg[:, h:h + 1]), r=['lg', 'rtab'], w=[('dc', h)])
        S.op('act', lambda e, h=h: e.activation(out=dc[:, h, 10:20], in_=rtab[:, 10:20], func=AF.Exp,
                                                scale=lg[:, 4 + h:5 + h]), r=['lg', 'rtab'], w=[('dc', h)])
        S.op('dve', lambda e, h=h: e.tensor_tensor(out=dc[:, h, 5:10], in0=dc[:, h, 5:10], in1=rtab[:, 20:25],
                                                   op=ALU.mult), r=[('dc', h), 'rtab'], w=[('dc', h)])
        S.op('dve', lambda e, h=h: e.tensor_tensor(out=dc[:, h, 15:20], in0=dc[:, h, 15:20], in1=rtab[:, 25:30],
                                                   op=ALU.mult), r=[('dc', h), 'rtab'], w=[('dc', h)])
        S.op('dve', lambda e, h=h: e.tensor_scalar(out=dc[:, h, 1:2], in0=dc[:, h, 1:2], scalar1=1.0 / 16,
                                                   scalar2=None, op0=ALU.mult), r=[('dc', h)], w=[('dc', h)])
        S.op('dve', lambda e, h=h: e.tensor_scalar(out=dc[:, h, 11:12], in0=dc[:, h, 11:12], scalar1=1.0 / 16,
                                                   scalar2=None, op0=ALU.mult), r=[('dc', h)], w=[('dc', h)])

    if stop == 'E0':
        dump('dc', dc[:], [128, 4, 20], [('dc', h_) for h_ in range(4)])
        return finish()
    own0 = 0
    for h in range(4):
        dck = ('dc', h)
        cols = [h * 256, 1024 + h * 256, 2048 + h * 256, 3072 + h * 256]
        for i in (1, 2, 0, 3):
            S.op('pool', lambda e, i=i, c=cols[i]: e.dma_start(
                out=ring[i][:], in_=win[:, c:c + 256].rearrange("(k p) n -> p k n", p=128)),
                w=[('ring', i, 0), ('ring', i, 1)], kind='d')
        def blk_dfb():
            S.op('act', lambda e, h=h: e.activation(out=e12[:, 0, :], in_=dmk[:, 0, :], func=AF.Exp, scale=lg[:, h:h + 1]),
                 r=['lg', 'dmk'], w=['e12'])
            S.op('act', lambda e, h=h: e.activation(out=e12[:, 1, :], in_=dmk[:, 1, :], func=AF.Exp,
                                                    scale=lg[:, 4 + h:5 + h]), r=['lg', 'dmk'], w=['e12'])
            S.op('dve', lambda e: e.tensor_tensor(out=e12[:], in0=e12[:], in1=dmk[:, 2:4, :], op=ALU.mult),
                 r=['e12', 'dmk'], w=['e12'])
            S.op('dve', lambda e: e.tensor_tensor(out=Dfb[:], in0=e12[:, 0, :], in1=e12[:, 1, :], op=ALU.add),
                 r=['e12'], w=['Dfb'])
        def blk_qk(dst, dkey, slot):
            for half in range(2):
                ba = 2 * half
                for blk in range(2):
                    for k in range(16):
                        S.op('pe', lambda e, k=k, blk=blk, half=half, slot=slot, ba=ba: e.matmul(
                            psb(ba + blk), lhsT=ring[slot][:, k, blk * 128:(blk + 1) * 128],
                            rhs=hTa[:, k, own0 + half * 512:own0 + (half + 1) * 512], start=(k == 0), stop=(k == 15)),
                            r=[('ring', slot, blk)] + hT_own, w=[pskey(ba + blk)])
                cs = ropet[:, 0, half * 512:(half + 1) * 512]
                sn = ropet[:, 1, half * 512:(half + 1) * 512]
                pa, pb = psb(ba), psb(ba + 1)
                S.op('dve', lambda e, pa=pa, cs=cs: e.tensor_tensor(out=rt[0][:], in0=pa, in1=cs, op=ALU.mult),
                     r=[pskey(ba), 'ropet'], w=[('rt', 0)])
                S.op('dve', lambda e, pb=pb, sn=sn: e.tensor_tensor(out=rt[1][:], in0=pb, in1=sn, op=ALU.mult),
                     r=[pskey(ba + 1), 'ropet'], w=[('rt', 1)])
                S.op('dve', lambda e, pa=pa, sn=sn: e.tensor_tensor(out=rt[2][:], in0=pa, in1=sn, op=ALU.mult),
                     r=[pskey(ba), 'ropet'], w=[('rt', 2)])
                S.op('dve', lambda e, pb=pb, cs=cs: e.tensor_tensor(out=rt[3][:], in0=pb, in1=cs, op=ALU.mult),
                     r=[pskey(ba + 1), 'ropet'], w=[('rt', 3)])
                S.op('dve', lambda e, dst=dst, half=half: e.tensor_sub(out=dst[:, 0, half * 512:(half + 1) * 512],
                                                                         in0=rt[0][:], in1=rt[1][:]),
                     r=[('rt', 0), ('rt', 1)], w=[(dkey, half)])
                S.op('dve', lambda e, dst=dst, half=half: e.tensor_add(out=dst[:, 1, half * 512:(half + 1) * 512],
                                                                         in0=rt[2][:], in1=rt[3][:]),
                     r=[('rt', 2), ('rt', 3)], w=[(dkey, half)])
        qk_keys = [('qT', 0), ('qT', 1), ('kT', 0), ('kT', 1)]
        def blk_ktm():
            zcol = small[:, 8:9]
            if h == 0:
                S.op('pool', lambda e: e.memset(small[:, 8:16], 0.0), w=['zcol'])
            for c in range(8):
                bank = 4 + c % 2
                pstv = PS[:, bank:bank + 1, :].rearrange("p b f -> p (b f)").bitcast(BF16).rearrange("p (k t) -> p k t", t=128)
                for blk in range(2):
                    S.op('pe', lambda e, c=c, blk=blk, pstv=pstv: e.transpose(pstv[:, blk, :],
                                                                             kT[:, blk, c * 128:(c + 1) * 128], ident[:]),
                         r=[('kT', c // 4), 'ident'], w=[pskey(bank)])
                for blk in range(2):
                    S.op('act', lambda e, c=c, blk=blk, pstv=pstv, h=h: e.activation(
                        out=ktm[0][:, c, blk * 128:(blk + 1) * 128], in_=pstv[:, blk, :], func=AF.Identity,
                        scale=dc[:, h, 0:1], bias=zcol), r=[pskey(bank), dck, 'zcol'], w=[('ktm', 0, c)])
                    S.op('dve', lambda e, c=c, blk=blk, pstv=pstv, h=h: e.tensor_scalar(
                        out=ktm[1][:, c, blk * 128:(blk + 1) * 128], in0=pstv[:, blk, :], scalar1=dc[:, h, 10:11],
                        scalar2=zcol, op0=ALU.mult, op1=ALU.add), r=[pskey(bank), dck, 'zcol'], w=[('ktm', 1, c)])
        def blk_vg(which_list):
            for c in range(8):
                for which, (slot, dstt, dk) in [(w_, [(2, vtm, 'vtm'), (3, gs, 'gs')][w_]) for w_ in which_list]:
                    bank = 6 + c % 2
                    for k in range(16):
                        S.op('pe', lambda e, k=k, c=c, slot=slot, bank=bank: e.matmul(
                            psb(bank)[:, 0:256], lhsT=hTa[:, k, own0 + c * 128:own0 + (c + 1) * 128], rhs=ring[slot][:, k, :],
                            start=(k == 0), stop=(k == 15)),
                            r=[('ring', slot, 0), ('ring', slot, 1), ('hT', 2 + c)], w=[pskey(bank)])
                    S.op('act', lambda e, c=c, dstt=dstt, bank=bank, which=which: e.activation(
                        out=dstt[:, c, :], in_=psb(bank)[:, 0:256], func=(AF.Copy if which == 0 else AF.Silu)),
                        r=[pskey(bank)], w=[(dk, c)])
        def blk_ctx():
            for t in range(2):
                for k in range(16):
                    S.op('pe', lambda e, k=k, t=t: e.matmul(
                        psb(4)[:, 0:256], lhsT=hTa[:, k, 1024 + t * 128:1024 + (t + 1) * 128], rhs=ring[1][:, k, :],
                        start=(k == 0), stop=(k == 15)),
                        r=[('ring', 1, 0), ('ring', 1, 1), ('hT', 12 + t)], w=[pskey(4)])
                S.op('act', lambda e, t=t, h=h: e.activation(out=cktm[0][:, t, :], in_=psb(4)[:, 0:256], func=AF.Copy,
                                                            scale=dc[:, h, 3 + t:4 + t]),
                     r=[pskey(4), dck], w=[('cktm', 0)])
                S.op('dve', lambda e, t=t, h=h: e.tensor_scalar(out=cktm[1][:, t, :], in0=psb(4)[:, 0:256],
                                                               scalar1=dc[:, h, 13 + t:14 + t], scalar2=None,
                                                               op0=ALU.mult),
                     r=[pskey(4), dck], w=[('cktm', 1)])
                for k in range(16):
                    S.op('pe', lambda e, k=k, t=t: e.matmul(
                        psb(5)[:, 0:256], lhsT=hTa[:, k, 1024 + t * 128:1024 + (t + 1) * 128], rhs=ring[2][:, k, :],
                        start=(k == 0), stop=(k == 15)),
                        r=[('ring', 2, 0), ('ring', 2, 1), ('hT', 12 + t)], w=[pskey(5)])
                S.op('act', lambda e, t=t: e.activation(out=cvtm[:, t, :], in_=psb(5)[:, 0:256], func=AF.Copy),
                     r=[pskey(5)], w=['cvtm'])
        ktm_keys = [[('ktm', d, c) for c in range(8)] for d in range(2)]
        vtm_keys = [('vtm', c) for c in range(8)]

        def recur(store):
            for d in range(2):
                order = range(8) if d == 0 else range(7, -1, -1)
                cd = dc[:, h, 2:3] if d == 0 else dc[:, h, 12:13]
                for c in order:
                    if store:
                        S.op('act', lambda e, d=d, c=c: e.activation(
                            out=Sbf[d][:, c, :, :].rearrange("p b v -> p (b v)"),
                            in_=St[d][:].rearrange("p b v -> p (b v)"), func=AF.Copy),
                            r=[('St', d)], w=[('Sbf', d, c)])
                    bank = c % 2
                    pm = psb(bank).rearrange("p (b v) -> p b v", v=256)
                    for blk in range(2):
                        S.op('pe', lambda e, d=d, c=c, blk=blk, pm=pm: e.matmul(
                            pm[:, blk, :], lhsT=ktm[d][:, c, blk * 128:(blk + 1) * 128], rhs=vtm[:, c, :],
                            start=True, stop=True), r=[('ktm', d, c), ('vtm', c)], w=[pskey(bank)])
                    S.op('dve', lambda e, d=d, cd=cd, bank=bank: e.scalar_tensor_tensor(
                        out=St[d][:].rearrange("p b v -> p (b v)"), in0=St[d][:].rearrange("p b v -> p (b v)"),
                        scalar=cd, in1=psb(bank), op0=ALU.mult, op1=ALU.add),
                        r=[('St', d), pskey(bank), dck], w=[('St', d)])

        blk_qk(kT, 'kT', 1)
        blk_ktm()
        blk_vg([0])
        blk_ctx()
        for d in range(2):
            S.op('pool', lambda e, d=d: e.memset(St[d][:], 0.0), w=[('St', d)])
        recur(False)
        for d in range(2):
            S.op('sp', lambda e, d=d, h=h: e.dma_start(
                out=stin[h].ap().rearrange("(a p) v -> p a v", p=128)[:, 2 * d:2 * d + 2, :], in_=St[d][:]),
                r=[('St', d)], w=[('stin', h)], kind='d')
        S.op('pool', lambda e, h=h: e.collective_compute(
            "AllGather", ALU.bypass, replica_groups=[[0, 1, 2, 3], [4, 5, 6, 7]],
            ins=[stin[h].ap().opt()], outs=[stall[h].ap().opt()]), r=[('stin', h)], w=[('stall', h)], kind='cc')
        blk_dfb()
        blk_qk(qT, 'qT', 0)
        blk_vg([1])
        for d in range(2):
            pc = psb(2 + d).rearrange("p (b v) -> p b v", v=256)
            for blk in range(2):
                for t in range(2):
                    S.op('pe', lambda e, d=d, blk=blk, t=t, pc=pc: e.matmul(
                        pc[:, blk, :], lhsT=cktm[d][:, t, blk * 128:(blk + 1) * 128], rhs=cvtm[:, t, :],
                        start=(t == 0), stop=(t == 1)), r=[('cktm', d), 'cvtm'], w=[pskey(2 + d)])
            c0 = 5 if d == 0 else 15
            S.op('dve', lambda e, d=d, c0=c0, h=h: e.tensor_scalar(
                out=St[d][:].rearrange("p b v -> p (b v)"), in0=psb(2 + d), scalar1=dc[:, h, c0:c0 + 1], scalar2=None,
                op0=ALU.mult), r=[pskey(2 + d), dck], w=[('St', d)])
        for r_ in range(4):
            S.op('sp', lambda e, h=h, r_=r_: e.dma_start(
                out=G[r_ % 2][:], in_=stall[h].ap()[r_ * 512:(r_ + 1) * 512, :].rearrange("(a p) v -> p a v", p=128)),
                r=[('stall', h)], w=[('G', r_ % 2)], kind='d')
            for d in range(2):
                c0 = 5 if d == 0 else 15
                S.op('dve', lambda e, d=d, r_=r_, c0=c0, h=h: e.scalar_tensor_tensor(
                    out=St[d][:], in0=G[r_ % 2][:, 2 * d:2 * d + 2, :], scalar=dc[:, h, c0 + 1 + r_:c0 + 2 + r_],
                    in1=St[d][:], op0=ALU.mult, op1=ALU.add), r=[('G', r_ % 2), ('St', d), dck], w=[('St', d)])
        if stop == 'E3':
            dump('St0', St[0][:], [128, 2, 256], [('St', 0)])
            dump('St1', St[1][:], [128, 2, 256], [('St', 1)])
            return finish()
        recur(True)
        def p2b_scores(c):
            p2 = c % 2
            pS = psb(4 + p2)[:, 0:128]
            for blk in range(2):
                S.op('pe', lambda e, blk=blk: e.matmul(
                    pS, lhsT=kT[:, blk, c * 128:(c + 1) * 128], rhs=qT[:, blk, c * 128:(c + 1) * 128],
                    start=(blk == 0), stop=(blk == 1)), r=qk_keys, w=[pskey(4 + p2)])
            S.op('dve', lambda e: e.tensor_tensor(out=PTr[p2][:], in0=pS, in1=Dfb[:], op=ALU.mult),
                 r=[pskey(4 + p2), 'Dfb'], w=[('PTr', p2)])

        p2b_scores(0)
        for c in range(8):
            p2 = c % 2
            if c + 1 < 8:
                p2b_scores(c + 1)
            pI = psb(6 + p2)[:, 0:256]
            pF = psb(6 + p2)[:, 256:512]
            pB = psb(p2)[:, 0:256]
            for blk in range(2):
                S.op('pe', lambda e, c=c, blk=blk, pF=pF: e.matmul(
                    pF, lhsT=qT[:, blk, c * 128:(c + 1) * 128], rhs=Sbf[0][:, c, blk, :], start=(blk == 0),
                    stop=(blk == 1)), r=qk_keys + [('Sbf', 0, c)], w=[pskey(6 + p2)])
            for blk in range(2):
                S.op('pe', lambda e, c=c, blk=blk, pB=pB: e.matmul(
                    pB, lhsT=qT[:, blk, c * 128:(c + 1) * 128], rhs=Sbf[1][:, c, blk, :], start=(blk == 0),
                    stop=(blk == 1)), r=qk_keys + [('Sbf', 1, c)], w=[pskey(p2)])
            S.op('pe', lambda e, c=c, p2=p2, pI=pI: e.matmul(pI, lhsT=PTr[p2][:], rhs=vtm[:, c, :], start=True,
                                                            stop=True),
                 r=[('PTr', p2), ('vtm', c)], w=[pskey(6 + p2)])
            S.op('act', lambda e, c=c, pI=pI: e.activation(out=oall[:, c, :], in_=pI, func=AF.Copy),
                 r=[pskey(6 + p2)], w=[('oall', c)])
            S.op('dve', lambda e, c=c, pF=pF, h=h: e.scalar_tensor_tensor(
                out=oall[:, c, :], in0=pF, scalar=dc[:, h, 1:2], in1=oall[:, c, :], op0=ALU.mult, op1=ALU.add),
                r=[pskey(6 + p2), ('oall', c), dck], w=[('oall', c)])
            S.op('dve', lambda e, c=c, pB=pB, h=h: e.scalar_tensor_tensor(
                out=oall[:, c, :], in0=pB, scalar=dc[:, h, 11:12], in1=oall[:, c, :], op0=ALU.mult, op1=ALU.add),
                r=[pskey(p2), ('oall', c), dck], w=[('oall', c)])
            S.op('dve', lambda e, c=c: e.bn_stats(out=bst[:, c, :], in_=oall[:, c, :]), r=[('oall', c)], w=[('bst', c)])
            S.op('dve', lambda e, c=c: e.bn_aggr(out=mv[:, c, :], in_=bst[:, c, :]), r=[('bst', c)], w=['mv'])
        if stop == 'E4':
            dump('oall', oall[:], [128, 8, 256], [('oall', c_) for c_ in range(8)])
            return finish()
        S.op('dve', lambda e: e.tensor_scalar(out=rstd_r[:], in0=mv[:, :, 1], scalar1=EPS, scalar2=None, op0=ALU.add),
             r=['mv'], w=['rstd_r'])
        S.op('act', lambda e: e.activation(out=rstd_r[:], in_=rstd_r[:], func=AF.Sqrt), r=['rstd_r'], w=['rstd_r'])
        S.op('dve', lambda e: e.reciprocal(out=rstd_r[:], in_=rstd_r[:]), r=['rstd_r'], w=['rstd_r'])
        for c in range(8):
            p2 = c % 2
            S.op('dve', lambda e, c=c, p2=p2: e.tensor_scalar(
                out=ynorm[p2][:], in0=oall[:, c, :], scalar1=mv[:, c, 0:1], scalar2=rstd_r[:, c:c + 1],
                op0=ALU.subtract, op1=ALU.mult), r=[('oall', c), 'mv', 'rstd_r'], w=[('yn', p2)])
            S.op('dve', lambda e, p2=p2, h=h: e.tensor_mul(out=ynorm[p2][:], in0=ynorm[p2][:],
                                                            in1=gnb[:, h * 256:(h + 1) * 256]),
                 r=[('yn', p2), 'gnb'], w=[('yn', p2)])
            S.op('dve', lambda e, c=c, p2=p2, h=h: e.tensor_mul(out=cat[:, c, h * 256:(h + 1) * 256],
                                                                 in0=ynorm[p2][:], in1=gs[:, c, :]),
                 r=[('yn', p2), ('gs', c)], w=[('cat', c)])

    dump('cat2', cat[:], [128, 8, 2048], [('cat', m) for m in range(8)], BF16)
    if stop == 'E':
        return finish()
    S.bar()

    A.seek(159, 207.5)
    gbc = A.alloc([128, 2, 2048], F32, "gbc")
    hfT = A.alloc([128, 16, 1024], BF16, "hfT")
    A.seek(38, 159)
    woutb = A.alloc([128, 16, 2048], BF16, "woutb")
    gB = A.alloc([128, 16, 128], F32, "gB")
    xr = [A.alloc([128, 2048], F32, "xr%d" % i) for i in range(2)]
    x1t = [A.alloc([128, 2048], F32, "x1t%d" % i) for i in range(2)]
    catT = [A.alloc([128, 16, 128], BF16, "catT%d" % i) for i in range(2)]
    xn2 = [A.alloc([128, 2048], BF16, "xn2%d" % i) for i in range(2)]

    for j in range(4):
        S.op('pool', lambda e, j=j: e.dma_start(
            out=woutb[:, :, j * 512:(j + 1) * 512],
            in_=wout[:, j * 512:(j + 1) * 512].rearrange("(k p) n -> p k n", p=128)), w=[('woutb', j)], kind='d')
    S.op('pool', lambda e: e.dma_start(out=wrb[:], in_=wrt.rearrange("(k p) n -> p k n", p=128)), w=['wrb'], kind='d')
    S.op('sp', lambda e: e.dma_start(out=brb[:], in_=brt.partition_broadcast(128)), w=['brb'], kind='d')
    for gi, base in enumerate([32, 80]):
        S.op('dve', lambda e, base=base: e.tensor_copy(
            out=gB[:], in_=modc[:, base:base + 16, 0:1].to_broadcast([128, 16, 128])),
            r=[('modc', 1, 0)], w=['gB'])
        for q4 in range(4):
            for kk in range(4):
                k = q4 * 4 + kk
                S.op('pe', lambda e, k=k, kk=kk: e.matmul(psb(0)[:, kk * 128:(kk + 1) * 128], lhsT=gB[:, k, :],
                                                        rhs=identf[:], start=True, stop=True),
                     r=['gB', 'identf'], w=[pskey(0)])
            S.op('act', lambda e, gi=gi, q4=q4: e.activation(out=gbc[:, gi, q4 * 512:(q4 + 1) * 512], in_=psb(0),
                                                            func=AF.Copy), r=[pskey(0)], w=[('gbc', gi)])
    S.op('pool', lambda e: e.memset(rsm[:], 0.0), w=['rsm'])

    def f_stage1(i):
        p2 = i % 2
        S.op('sp', lambda e, i=i, p2=p2: e.dma_start(out=xr[p2][:], in_=xs[256 + i * 128:256 + (i + 1) * 128, :]),
             w=[('xr', p2)], kind='d')
        pst = PS[:, 2:4, :].rearrange("p b f -> p (b f)").bitcast(BF16).rearrange("p (k t) -> p k t", t=128)
        for k in range(16):
            S.op('pe', lambda e, k=k, i=i, pst=pst: e.transpose(pst[:, k, :], cat[:, i, k * 128:(k + 1) * 128], ident[:]),
                 r=[('cat', i), 'ident'], w=[pskey(2 + k // 8)])
        S.op('dve', lambda e, p2=p2, pst=pst: e.tensor_copy(out=catT[p2][:, 0:8, :], in_=pst[:, 0:8, :]),
             r=[pskey(2)], w=[('catT', p2, 0)])
        S.op('act', lambda e, p2=p2, pst=pst: e.activation(out=catT[p2][:, 8:16, :], in_=pst[:, 8:16, :], func=AF.Copy),
             r=[pskey(3)], w=[('catT', p2, 1)])
        for j in range(4):
            bank = 4 + j % 2
            for k in range(16):
                S.op('pe', lambda e, k=k, j=j, p2=p2, bank=bank: e.matmul(
                    psb(bank), lhsT=catT[p2][:, k, :], rhs=woutb[:, k, j * 512:(j + 1) * 512], start=(k == 0),
                    stop=(k == 15)), r=[('catT', p2, k // 8), ('woutb', j)], w=[pskey(bank)])
            S.op('dve', lambda e, j=j, p2=p2, bank=bank: e.tensor_tensor(
                out=x1t[p2][:, j * 512:(j + 1) * 512], in0=psb(bank), in1=gbc[:, 0, j * 512:(j + 1) * 512], op=ALU.mult),
                r=[pskey(bank), ('gbc', 0)], w=[('x1t', p2, j)])
            S.op('dve', lambda e, j=j, p2=p2: e.tensor_add(
                out=x1t[p2][:, j * 512:(j + 1) * 512], in0=x1t[p2][:, j * 512:(j + 1) * 512],
                in1=xr[p2][:, j * 512:(j + 1) * 512]), r=[('x1t', p2, j), ('xr', p2)], w=[('x1t', p2, j)])
        x1keys = [('x1t', p2, j) for j in range(4)]
        S.op('pool', lambda e, i=i, p2=p2: e.dma_start(out=x1s[i * 128:(i + 1) * 128, :], in_=x1t[p2][:]),
             r=x1keys, w=[('x1s', i)], kind='d')
        sc_, rc_ = rsm[:, i:i + 1], rsm[:, 8 + i:9 + i]
        S.op('act', lambda e, p2=p2, sc_=sc_: e.activation(out=xn2[p2][:], in_=x1t[p2][:], func=AF.Square, accum_out=sc_),
             r=x1keys + ['rsm'], w=[('xn2', p2), ('F', i, 'ss')])
        S.op('dve', lambda e, sc_=sc_, rc_=rc_: e.tensor_scalar(out=rc_, in0=sc_, scalar1=1.0 / 2048, scalar2=EPS,
                                                               op0=ALU.mult, op1=ALU.add),
             r=[('F', i, 'ss')], w=[('F', i, 'rs')])
        S.op('act', lambda e, rc_=rc_: e.activation(out=rc_, in_=rc_, func=AF.Sqrt), r=[('F', i, 'rs')],
             w=[('F', i, 'rs')])
        S.op('dve', lambda e, rc_=rc_: e.reciprocal(out=rc_, in_=rc_), r=[('F', i, 'rs')], w=[('F', i, 'rs')])
        S.op('dve', lambda e, p2=p2, rc_=rc_: e.tensor_scalar(out=xn2[p2][:], in0=x1t[p2][:], scalar1=rc_, scalar2=None,
                                                              op0=ALU.mult), r=x1keys + [('F', i, 'rs')],
             w=[('xn2', p2)])
    def f_stage2(i):
        p2 = i % 2
        transpose_mod(xn2[p2], ('xn2', p2),
                      lambda k, i=i: hfT[:, k, i * 128:(i + 1) * 128],
                      lambda k, i=i: ('hfT', i),
                      lambda k: ABc[:, 2, k:k + 1],
                      lambda k: modc[:, 48 + k, 0:1],
                      'A2', ('modc', 1, 0), 6)
        for k in range(16):
            S.op('pe', lambda e, k=k, i=i: e.matmul(psb(1)[:, 0:36], lhsT=hfT[:, k, i * 128:(i + 1) * 128],
                                                   rhs=wrb[:, k, :], start=(k == 0), stop=(k == 15)),
                 r=[('hfT', i), 'wrb'], w=[pskey(1)])
        R = 'rtr'

        def dv(fn, r, w):
            S.op('dve', fn, r=[(R, x) for x in r], w=[(R, x) for x in w])
        sm = rsm[:, 16:64]
        gmax, ngmax, gsum, gw = sm[:, 0:1], sm[:, 1:2], sm[:, 2:3], sm[:, 3:4]
        goh, gex = sm[:, 4:8], sm[:, 8:12]
        esel, mx8 = sm[:, 12:20], sm[:, 20:28]
        mk1, mk2 = sm[:, 28:36], sm[:, 36:44]
        dd, ee, w1, w2 = sm[:, 44:45], sm[:, 45:46], sm[:, 46:47], sm[:, 47:48]
        S.op('dve', lambda e: e.tensor_tensor(out=lgt[:], in0=psb(1)[:, 0:36], in1=brb[:], op=ALU.add),
             r=[pskey(1), 'brb'], w=[(R, 'lgt')])
        dv(lambda e: e.reduce_max(out=gmax, in_=lgt[:, 0:4], axis=AX.X), ['lgt'], ['gmax'])
        dv(lambda e: e.tensor_scalar(out=goh, in0=lgt[:, 0:4], scalar1=gmax, scalar2=None, op0=ALU.is_equal),
           ['lgt', 'gmax'], ['goh'])
        dv(lambda e: e.tensor_scalar(out=ngmax, in0=gmax, scalar1=-1.0, scalar2=None, op0=ALU.mult), ['gmax'], ['ngmax'])
        dv(lambda e: e.memset(gsum, 0.0), [], ['gsum'])
        S.op('act', lambda e: e.activation(out=gex, in_=lgt[:, 0:4], func=AF.Exp, bias=ngmax, accum_out=gsum),
             r=[(R, 'lgt'), (R, 'ngmax'), (R, 'gsum')], w=[(R, 'gsum'), (R, 'gex')])
        dv(lambda e: e.reciprocal(out=gw, in_=gsum), ['gsum'], ['gw'])
        dv(lambda e: e.tensor_scalar(out=esel, in0=lgt[:, 4:12], scalar1=goh[:, 0:1], scalar2=None, op0=ALU.mult),
           ['lgt', 'goh'], ['esel'])
        for g in range(1, 4):
            dv(lambda e, g=g: e.scalar_tensor_tensor(out=esel, in0=lgt[:, 4 + 8 * g:12 + 8 * g], scalar=goh[:, g:g + 1],
                                                     in1=esel, op0=ALU.mult, op1=ALU.add), ['lgt', 'goh', 'esel'],
               ['esel'])
        dv(lambda e: e.max(out=mx8, in_=esel), ['esel'], ['mx8'])
        dv(lambda e: e.tensor_scalar(out=mk1, in0=esel, scalar1=mx8[:, 0:1], scalar2=None, op0=ALU.is_equal),
           ['esel', 'mx8'], ['mk1'])
        dv(lambda e: e.tensor_scalar(out=mk2, in0=esel, scalar1=mx8[:, 1:2], scalar2=None, op0=ALU.is_equal),
           ['esel', 'mx8'], ['mk2'])
        dv(lambda e: e.tensor_sub(out=dd, in0=mx8[:, 1:2], in1=mx8[:, 0:1]), ['mx8'], ['dd'])
        S.op('act', lambda e: e.activation(out=ee, in_=dd, func=AF.Exp), r=[(R, 'dd')], w=[(R, 'ee')])
        dv(lambda e: e.tensor_scalar(out=w1, in0=ee, scalar1=1.0, scalar2=None, op0=ALU.add), ['ee'], ['w1'])
        dv(lambda e: e.reciprocal(out=w1, in_=w1), ['w1'], ['w1'])
        dv(lambda e: e.tensor_mul(out=w2, in0=ee, in1=w1), ['ee', 'w1'], ['w2'])
        dv(lambda e: e.tensor_mul(out=w1, in0=w1, in1=gw), ['w1', 'gw'], ['w1'])
        dv(lambda e: e.tensor_mul(out=w2, in0=w2, in1=gw), ['w2', 'gw'], ['w2'])
        dv(lambda e: e.tensor_scalar(out=mk1, in0=mk1, scalar1=w1, scalar2=None, op0=ALU.mult), ['mk1', 'w1'], ['mk1'])
        dv(lambda e: e.scalar_tensor_tensor(out=mk1, in0=mk2, scalar=w2, in1=mk1, op0=ALU.mult, op1=ALU.add),
           ['mk1', 'mk2', 'w2'], ['mk1'])
        for g in range(4):
            S.op('dve', lambda e, g=g, i=i: e.tensor_scalar(out=comb[:, i, 8 * g:8 * g + 8], in0=mk1,
                                                           scalar1=goh[:, g:g + 1], scalar2=None, op0=ALU.mult),
                 r=[(R, 'mk1'), (R, 'goh')], w=[('comb', i)])
    for i in range(9):
        if i < 8:
            f_stage1(i)
        if i >= 1:
            f_stage2(i - 1)
    dump('hfT', hfT[:], [128, 16, 1024], [('hfT', i) for i in range(8)], BF16)
    dump('comb', comb[:], [128, 8, 32], [('comb', i) for i in range(8)])
    dump('gbc', gbc[:], [128, 2, 2048], [('gbc', 0), ('gbc', 1)])
    if stop == 'F':
        return finish()
    S.bar()
    A.seek(6, 159)

    acc = A.alloc([128, 8, 2048], F32, "acc")
    wring = [A.alloc([128, 8192], BF16, "wring%d" % i) for i in range(4)]
    aT = [A.alloc([128, 4, 1024], BF16, "aT%d" % i) for i in range(2)]
    sg = [A.alloc([128, 512], F32, "sg%d" % i) for i in range(2)]
    hf_keys = [('hfT', i) for i in range(8)]
    wi = [0]

    def wslot():
        s = wi[0] % 4
        wi[0] += 1
        return s
    pcnt = [0]
    for ex in range(32):
        sg_, su_, sd_ = wslot(), wslot(), wslot()
        Wg = wring[sg_][:].rearrange("p (k n) -> p k n", n=512)
        Wu = wring[su_][:].rearrange("p (k n) -> p k n", n=512)
        Wd = wring[sd_][:].rearrange("p (k n) -> p k n", n=2048)
        S.op('pool', lambda e, Wg=Wg, ex=ex: e.dma_start(out=Wg, in_=wg[ex].rearrange("(k p) n -> p k n", p=128)),
             w=[('wr', sg_)], kind='d')
        S.op('pool', lambda e, Wu=Wu, ex=ex: e.dma_start(out=Wu, in_=wu[ex].rearrange("(k p) n -> p k n", p=128)),
             w=[('wr', su_)], kind='d')
        S.op('pool', lambda e, Wd=Wd, ex=ex: e.dma_start(out=Wd, in_=wd[ex].rearrange("(k p) n -> p k n", p=128)),
             w=[('wr', sd_)], kind='d')
        ab = ex % 2
        for f in range(4):
            for th in range(2):
                bg = (pcnt[0] % 2) * 2
                pcnt[0] += 1
                for wi_, (W_, sk) in enumerate([(Wg, sg_), (Wu, su_)]):
                    for k in range(16):
                        S.op('pe', lambda e, W_=W_, k=k, f=f, th=th, bg=bg, wi_=wi_: e.matmul(
                            psb(bg + wi_), lhsT=W_[:, k, f * 128:(f + 1) * 128], rhs=hfT[:, k, th * 512:(th + 1) * 512],
                            start=(k == 0), stop=(k == 15)), r=[('wr', sk)] + hf_keys[th * 4:th * 4 + 4],
                            w=[pskey(bg + wi_)])
                sgi = pcnt[0] % 2
                S.op('act', lambda e, bg=bg, sgi=sgi: e.activation(out=sg[sgi][:], in_=psb(bg), func=AF.Silu),
                     r=[pskey(bg)], w=[('sg', sgi)])
                S.op('dve', lambda e, bg=bg, sgi=sgi, f=f, th=th, ab=ab: e.tensor_tensor(
                    out=aT[ab][:, f, th * 512:(th + 1) * 512], in0=psb(bg + 1), in1=sg[sgi][:], op=ALU.mult),
                    r=[pskey(bg + 1), ('sg', sgi)], w=[('aT', ab, th)])
        for i in range(8):
            for j in range(4):
                bank = 4 + (i * 4 + j) % 4
                for k in range(4):
                    S.op('pe', lambda e, k=k, i=i, j=j, bank=bank, ab=ab, Wd=Wd: e.matmul(
                        psb(bank), lhsT=aT[ab][:, k, i * 128:(i + 1) * 128], rhs=Wd[:, k, j * 512:(j + 1) * 512],
                        start=(k == 0), stop=(k == 3)), r=[('aT', ab, i // 4), ('wr', sd_)], w=[pskey(bank)])
                if ex == 0:
                    S.op('dve', lambda e, i=i, j=j, bank=bank, ex=ex: e.tensor_scalar(
                        out=acc[:, i, j * 512:(j + 1) * 512], in0=psb(bank), scalar1=comb[:, i, ex:ex + 1], scalar2=None,
                        op0=ALU.mult), r=[pskey(bank), ('comb', i)], w=[('acc', i, j)])
                else:
                    S.op('dve', lambda e, i=i, j=j, bank=bank, ex=ex: e.scalar_tensor_tensor(
                        out=acc[:, i, j * 512:(j + 1) * 512], in0=psb(bank), scalar=comb[:, i, ex:ex + 1],
                        in1=acc[:, i, j * 512:(j + 1) * 512], op0=ALU.mult, op1=ALU.add),
                        r=[pskey(bank), ('comb', i), ('acc', i, j)], w=[('acc', i, j)])

    S.bar()
    A.seek(6 + 64, 159)
    x1r = [A.alloc([128, 2048], F32, "x1r%d" % i) for i in range(2)]
    yo = [A.alloc([128, 2048], F32, "yo%d" % i) for i in range(2)]
    w3b = A.alloc([128, 2048], F32, "w3b")
    junk3 = A.alloc([128, 2048], BF16, "junk3")
    S.op('sp', lambda e: e.dma_start(out=w3b[:], in_=fnw.partition_broadcast(128)), w=['w3b'], kind='d')
    S.op('pool', lambda e: e.memset(rsm[:, 0:16], 0.0), w=['rsm2'])
    outs = []
    def x1r_load(i):
        p2 = i % 2
        S.op('sp', lambda e: e.dma_start(out=x1r[p2][:], in_=x1s[i * 128:(i + 1) * 128, :]),
             r=[('x1s', i)], w=[('x1r', p2)], kind='d')

    x1r_load(0)
    for i in range(8):
        p2 = i % 2
        if i + 1 < 8:
            x1r_load(i + 1)
        acck = [('acc', i, j) for j in range(4)]
        S.op('dve', lambda e, i=i, p2=p2: e.tensor_tensor(out=yo[p2][:], in0=acc[:, i, :], in1=gbc[:, 1, :], op=ALU.mult),
             r=acck + [('gbc', 1)], w=[('yo', p2)])
        S.op('dve', lambda e, p2=p2: e.tensor_add(out=yo[p2][:], in0=yo[p2][:], in1=x1r[p2][:]),
             r=[('yo', p2), ('x1r', p2)], w=[('yo', p2)])
        sc_, rc_ = rsm[:, i:i + 1], rsm[:, 8 + i:9 + i]
        S.op('act', lambda e, p2=p2, sc_=sc_: e.activation(out=junk3[:], in_=yo[p2][:], func=AF.Square, accum_out=sc_),
             r=[('yo', p2), 'rsm2'], w=['junk3', ('I', i, 'ss')])
        S.op('dve', lambda e, sc_=sc_, rc_=rc_: e.tensor_scalar(out=rc_, in0=sc_, scalar1=1.0 / 2048, scalar2=EPS,
                                                               op0=ALU.mult, op1=ALU.add),
             r=[('I', i, 'ss')], w=[('I', i, 'rs')])
        S.op('act', lambda e, rc_=rc_: e.activation(out=rc_, in_=rc_, func=AF.Sqrt), r=[('I', i, 'rs')],
             w=[('I', i, 'rs')])
        S.op('dve', lambda e, rc_=rc_: e.reciprocal(out=rc_, in_=rc_), r=[('I', i, 'rs')], w=[('I', i, 'rs')])
        S.op('dve', lambda e, p2=p2, rc_=rc_: e.scalar_tensor_tensor(
            out=yo[p2][:], in0=yo[p2][:], scalar=rc_, in1=w3b[:], op0=ALU.mult, op1=ALU.mult),
            r=[('yo', p2), ('I', i, 'rs'), 'w3b'], w=[('yo', p2)])
        outs.append(S.op('sp', lambda e, i=i, p2=p2: e.dma_start(out=y[i * 128:(i + 1) * 128, :], in_=yo[p2][:]),
                         r=[('yo', p2)], w=[('y', i)], kind='d'))
    return finish()


_PROG = {}


def _col(v):
    return np.ascontiguousarray(np.asarray(v, np.float32).reshape(-1, 128).T)


def _host_tables(na_rpb):
    rpb = np.asarray(na_rpb, np.float32)
    a = np.arange(2)[:, None, None, None, None]
    kc = np.arange(64)[None, :, None, None, None]
    bq = np.arange(2)[None, None, None, :, None]
    cq = np.arange(64)[None, None, None, None, :]
    cs = np.clip(cq - 8, 0, 48)
    colok = (kc >= cs) & (kc <= cs + 15)
    coff = np.clip(kc - cq + 15, 0, 30)
    tabs = []
    for s in range(4):
        r0 = 16 * s
        T = np.full((8, 8, 2, 64, 6, 2, 64), NEG, np.float32)
        for m in range(8):
            nl = nl_list(m)
            n = np.array(nl)[None, None, :, None, None]
            u = 2 * n + a
            rho = 2 * m + bq
            kr = r0 - 4 + u
            r = r0 + rho
            rsr = np.clip(r - 4, 0, 56)
            rowok = (kr >= rsr) & (kr <= rsr + 7) & (kr >= 0) & (kr <= 63)
            roff = np.clip(kr - r + 7, 0, 14)
            ok = np.broadcast_to(rowok & colok, (2, 64, 6, 2, 64))
            ro = np.broadcast_to(roff, (2, 64, 6, 2, 64))
            co = np.broadcast_to(coff, (2, 64, 6, 2, 64))
            for h in range(8):
                T[h, m] = np.where(ok, rpb[h][ro, co], np.float32(NEG))
        tabs.append(T.reshape(8, 8, 128, 6, 128))
    return tabs


def _const_tables():
    p = np.arange(128, dtype=np.float64)
    inv = 10000.0 ** (-(np.arange(64, dtype=np.float64)) / 64.0)
    ropes = []
    for s in range(4):
        t = np.arange(1024)
        row = 16 * s + t // 64
        col = t % 64
        ang = np.zeros((128, 1024))
        ang[0:64] = inv[:, None] * row[None, :]
        ang[64:128] = inv[:, None] * col[None, :]
        ropes.append(np.stack([np.cos(ang), np.sin(ang)], axis=1).astype(np.float32))
    rtabs = []
    for s in range(4):
        R = np.zeros((128, 30), np.float64)
        R[:, 0] = 127 - p
        R[:, 1] = p + 1
        R[:, 2] = 128
        R[:, 3] = 255 - p
        R[:, 4] = 127 - p
        R[:, 10] = p
        R[:, 11] = 128 - p
        R[:, 12] = 128
        R[:, 13] = p
        R[:, 14] = 128 + p
        R[:, 5] = 1024 * s
        R[:, 20] = 1.0
        R[:, 15] = 1024 * (3 - s)
        R[:, 25] = 1.0
        for i in range(4):
            if i < s:
                R[:, 6 + i] = 1024 * (s - 1 - i)
                R[:, 21 + i] = 1.0
            if i > s:
                R[:, 16 + i] = 1024 * (i - s - 1)
                R[:, 26 + i] = 1.0
        rtabs.append(R.astype(np.float32))
    j = np.arange(128)[:, None]
    i = np.arange(128)[None, :]
    dm = np.zeros((128, 4, 128), np.float32)
    dm[:, 0, :] = np.maximum(i - j, 0)
    dm[:, 1, :] = np.maximum(j - i, 0)
    dm[:, 2, :] = (i >= j) / 16.0
    dm[:, 3, :] = (j >= i) / 16.0
    return ropes, rtabs, dm


def kernel(x, c, ctx, c_ctx, w_mod, b_mod, norm_mix_w, w_in, ret_decay_f, ret_decay_b, ret_gn_w, na_rpb, w_out,
           norm_ffn_w, w_router_group, b_router_group, w_router_expert, b_router_expert, w_gate, w_up, w_down,
           final_norm_w):
    if 'nc' not in _PROG:
        _PROG['nc'] = build_program()
    nc = _PROG['nc']
    in_maps = make_inputs(x, c, ctx, c_ctx, w_mod, b_mod, norm_mix_w, w_in, ret_decay_f, ret_decay_b, ret_gn_w, na_rpb,
                          w_out, norm_ffn_w, w_router_group, b_router_group, w_router_expert, b_router_expert, w_gate,
                          w_up, w_down, final_norm_w)
    res = run_bass_kernel_spmd(nc, in_maps, core_ids=list(range(8)))
    out = np.zeros((2, 4096, 2048), np.float32)
    for core in range(8):
        b, s = core // 4, core % 4
        out[b, s * 1024:(s + 1) * 1024] = res.results[core]["y"]
    return out


def make_inputs(x, c, ctx, c_ctx, w_mod, b_mod, norm_mix_w, w_in, ret_decay_f, ret_decay_b, ret_gn_w, na_rpb, w_out,
                norm_ffn_w, w_router_group, b_router_group, w_router_expert, b_router_expert, w_gate, w_up, w_down,
                final_norm_w):
    f = lambda a: np.asarray(a, np.float32)
    x, c, ctx, c_ctx = f(x), f(c), f(ctx), f(c_ctx)
    perm = np.arange(7168)
    for base in list(range(0, 1024, 256)) + list(range(1024, 2048, 256)):
        perm[base:base + 256] = base + np.concatenate([np.arange(0, 64), np.arange(128, 192), np.arange(64, 128),
                                                       np.arange(192, 256)])
    win = np.ascontiguousarray(f(w_in)[0][:, perm])
    wmod = f(w_mod)[0]
    wout = f(w_out)[0]
    wrt = np.ascontiguousarray(np.concatenate(
        [f(w_router_group)[0], np.transpose(f(w_router_expert)[0], (1, 0, 2)).reshape(2048, 32)], axis=1))
    brt = np.concatenate([f(b_router_group)[0].reshape(-1), f(b_router_expert)[0].reshape(-1)])
    wg, wu, wd = f(w_gate)[0], f(w_up)[0], f(w_down)[0]
    fnw = f(final_norm_w)
    gnw = f(ret_gn_w)[0]
    dec = np.concatenate([f(ret_decay_f)[0], f(ret_decay_b)[0]])
    tcs = _host_tables(f(na_rpb)[0])
    ropes, rtabs, dm = _const_tables()
    bmodc = _col(f(b_mod)[0])
    nw1c = _col(f(norm_mix_w)[0])
    nw2c = _col(f(norm_ffn_w)[0])
    in_maps = []
    for core in range(8):
        b, s = core // 4, core % 4
        slab = np.zeros((24, 64, 2048), np.float32)
        xb = x[b].reshape(64, 64, 2048)
        lo, hi = 16 * s - 4, 16 * s + 20
        slo, shi = max(lo, 0), min(hi, 64)
        slab[slo - lo:shi - lo] = xb[slo:shi]
        ccol = np.stack([_col(c[b]), _col(c_ctx)], axis=2).reshape(128, 32)
        colp = np.ascontiguousarray(np.concatenate([ccol, bmodc, nw1c, nw2c], axis=1))
        in_maps.append({
            "xs": slab.reshape(1536, 2048), "ctxb": np.ascontiguousarray(ctx[b]), "colp": colp, "wmod": wmod,
            "win": win, "wout": wout, "wrt": wrt, "brt": brt, "wg": wg, "wu": wu, "wd": wd, "fnw": fnw, "gnw": gnw,
            "dec": dec, "ropet": ropes[s], "tcd": tcs[s], "rtab": rtabs[s], "dmk": dm,
        })
    return in_maps
```
